# Optimizing a Trainium2 kernel written in Bass

```python
import math
import jax, jax.numpy as jnp
from jax import lax
import numpy as np

D_MODEL = 1024
BATCH = 16
SEQ = 4096
DEPTH = 4

CTX_LEN = 256
GRID_W = 64
HEAD_DIM = 64
EPS = 1e-6
GDN_HEADS = 4
GDN_DK = 64
GDN_DV = 64
GDN_CONV = 3
GDN_CHUNK = 64
ATT_HEADS = 8
ATT_KV_HEADS = 2
ATT_GROUPS = ATT_HEADS // ATT_KV_HEADS
Q_BLOCK = 128
ROPE_THETA = 10000.0
SC_CH = 256
SC_CONV = 3
MIX_WIDTH = GDN_HEADS * GDN_DV + ATT_HEADS * HEAD_DIM + SC_CH
D_FF = 2816
N_EXPERTS = 8
TOP_K = 2
D_FF_EXPERT = 1408
N_DENSE = (DEPTH + 1) // 2
N_MOE = DEPTH // 2
A_QKV = GDN_HEADS * (2 * GDN_DK + GDN_DV)
A_Z = GDN_HEADS * GDN_DV
_SIZES = (A_QKV, A_Z, 2 * GDN_HEADS, 2 * GDN_HEADS,
          ATT_HEADS * HEAD_DIM, ATT_KV_HEADS * HEAD_DIM, ATT_KV_HEADS * HEAD_DIM,
          SC_CH, SC_CH, SC_CH)
IN_COLS = sum(_SIZES)
SPLIT_IDX = tuple(sum(_SIZES[:i + 1]) for i in range(len(_SIZES) - 1))

kernel_name = "hybrid_parallel_gdn_gqa_shortconv_moe_dit"

F32 = jnp.float32


def rmsnorm(x, g):
    xf = x.astype(F32)
    y = xf * lax.rsqrt(jnp.mean(jnp.square(xf), axis=-1, keepdims=True) + EPS)
    return (y * g.astype(F32)).astype(x.dtype)


def l2norm(x):
    return x * lax.rsqrt(jnp.sum(jnp.square(x), axis=-1, keepdims=True) + EPS)


def modulate(h, shift, scale):
    return h * (1 + scale) + shift


def dwconv(x, w):
    k = w.shape[0]
    return lax.conv_general_dilated(x, w[:, None, :].astype(x.dtype), (1,), [(k // 2, k // 2)],
                                    dimension_numbers=('NWC', 'WIO', 'NWC'),
                                    feature_group_count=x.shape[-1])


def grid_positions(n):
    rows = n // GRID_W
    t = jnp.arange(rows * GRID_W, dtype=jnp.int32)
    return t // GRID_W, t % GRID_W


def axial_rope(x, row, col):
    half = x.shape[-1] // 2
    quarter = half // 2
    inv_freq = ROPE_THETA ** (-jnp.arange(quarter, dtype=F32) / quarter)

    def rotate(xa, pos):
        ang = pos.astype(F32)[:, None] * inv_freq[None, :]
        cos = jnp.cos(ang)[None, :, None, :]
        sin = jnp.sin(ang)[None, :, None, :]
        x1, x2 = xa[..., :quarter], xa[..., quarter:]
        return jnp.concatenate([x1 * cos - x2 * sin, x2 * cos + x1 * sin], axis=-1)

    xf = x.astype(F32)
    return jnp.concatenate([rotate(xf[..., :half], row), rotate(xf[..., half:], col)], axis=-1).astype(x.dtype)


def block_attention(q, k, v):
    b, lq, _, dh = q.shape
    nb = lq // Q_BLOCK
    qb = jnp.moveaxis(q.reshape(b, nb, Q_BLOCK, ATT_KV_HEADS, ATT_GROUPS, dh), 1, 0)
    scale = dh ** -0.5

    def one_block(qi):
        s = jnp.einsum('bqkgd,bskd->bkgqs', qi, k).astype(F32) * scale
        p = jax.nn.softmax(s, axis=-1).astype(v.dtype)
        return jnp.einsum('bkgqs,bskd->bqkgd', p, v)

    o = lax.map(one_block, qb)
    return jnp.moveaxis(o, 0, 1).reshape(b, lq, ATT_HEADS * dh)


def gated_delta_chunked(q, k, v, log_a, beta, s0):
    b, h, l, dk = q.shape
    dv = v.shape[-1]
    cs = GDN_CHUNK
    n = l // cs
    q = (q * dk ** -0.5).reshape(b, h, n, cs, dk)
    k = k.reshape(b, h, n, cs, dk)
    v = v.reshape(b, h, n, cs, dv)
    beta = beta.reshape(b, h, n, cs)
    gc = jnp.cumsum(log_a.reshape(b, h, n, cs), axis=-1)
    incl = jnp.tril(jnp.ones((cs, cs), dtype=bool))
    strict = jnp.tril(jnp.ones((cs, cs), dtype=bool), -1)
    decay = jnp.where(incl, jnp.exp(jnp.where(incl, gc[..., :, None] - gc[..., None, :], 0.0)), 0.0)
    kk = jnp.einsum('bhnid,bhnjd->bhnij', k, k)
    lower = jnp.where(strict, beta[..., :, None] * kk * decay, 0.0)
    rhs = jnp.concatenate([v * beta[..., None], k * (beta * jnp.exp(gc))[..., None]], axis=-1)
    sol = lax.linalg.triangular_solve(jnp.eye(cs, dtype=F32) + lower, rhs,
                                      left_side=True, lower=True, unit_diagonal=True)
    u, w = sol[..., :dv], sol[..., dv:]
    qk = jnp.where(incl, jnp.einsum('bhnid,bhnjd->bhnij', q, k) * decay, 0.0)
    q_dec = q * jnp.exp(gc)[..., None]
    k_dec = k * jnp.exp(gc[..., -1:] - gc)[..., None]
    a_last = jnp.exp(gc[..., -1])

    def step(s, xs):
        qk_i, u_i, w_i, qd_i, kd_i, al_i = xs
        v_new = u_i - jnp.einsum('bhik,bhkv->bhiv', w_i, s)
        o = jnp.einsum('bhik,bhkv->bhiv', qd_i, s) + jnp.einsum('bhij,bhjv->bhiv', qk_i, v_new)
        s = s * al_i[..., None, None] + jnp.einsum('bhik,bhiv->bhkv', kd_i, v_new)
        return s, o

    xs = tuple(jnp.moveaxis(t, 2, 0) for t in (qk, u, w, q_dec, k_dec, a_last))
    s_fin, o = lax.scan(step, s0, xs)
    return jnp.moveaxis(o, 0, 2).reshape(b, h, l, dv), s_fin


def gdn_inputs(qkv, a, bt, conv_w, a_log, dt_bias):
    b, l, _ = qkv.shape
    qkv = jax.nn.silu(dwconv(qkv, conv_w)).astype(F32)
    q, k, v = jnp.split(qkv, [GDN_HEADS * GDN_DK, 2 * GDN_HEADS * GDN_DK], axis=-1)

    def heads(t, d):
        return jnp.swapaxes(t.reshape(b, l, GDN_HEADS, d), 1, 2)

    q = l2norm(heads(q, GDN_DK))
    k = l2norm(heads(k, GDN_DK))
    v = heads(v, GDN_DV)
    a = a.astype(F32).reshape(b, l, 2, GDN_HEADS).transpose(2, 0, 3, 1)
    bt = bt.astype(F32).reshape(b, l, 2, GDN_HEADS).transpose(2, 0, 3, 1)
    a_log = a_log.astype(F32)[:, None, :, None]
    dt_bias = dt_bias.astype(F32)[:, None, :, None]
    log_a = -jnp.exp(a_log) * jax.nn.softplus(a + dt_bias)
    beta = jax.nn.sigmoid(bt)
    return q, k, v, log_a, beta


def gdn_bidirectional(ctx_in, lat_in):
    qc, kc, vc, lac, bec = ctx_in
    ql, kl, vl, lal, bel = lat_in
    b = ql.shape[0]
    o_ctx = 0.0
    o_lat = 0.0
    for d in range(2):
        f = (lambda t: jnp.flip(t, axis=2)) if d == 1 else (lambda t: t)
        s0 = jnp.zeros((b, GDN_HEADS, GDN_DK, GDN_DV), F32)
        oc, s_ctx = gated_delta_chunked(f(qc), f(kc), f(vc), f(lac[d]), f(bec[d]), s0)
        ol, _ = gated_delta_chunked(f(ql), f(kl), f(vl), f(lal[d]), f(bel[d]), s_ctx)
        o_ctx = o_ctx + f(oc)
        o_lat = o_lat + f(ol)
    return o_ctx, o_lat


def gdn_output(o, z, g):
    b, h, l, dv = o.shape
    o = rmsnorm(jnp.swapaxes(o, 1, 2), g) * jax.nn.silu(z.astype(F32)).reshape(b, l, h, dv)
    return o.reshape(b, l, h * dv).astype(z.dtype)


def attn_heads(q, k, v, qn, kn):
    b, l, _ = q.shape
    q = rmsnorm(q.reshape(b, l, ATT_HEADS, HEAD_DIM), qn)
    k = rmsnorm(k.reshape(b, l, ATT_KV_HEADS, HEAD_DIM), kn)
    v = v.reshape(b, l, ATT_KV_HEADS, HEAD_DIM)
    return q, k, v


def short_conv_mix(bg, cg, hh, w):
    return bg * dwconv(cg * hh, w)


def token_mixers(hl, hc, w_in, conv_a, a_log, dt_bias, gdn_g, q_n, k_n, conv_c, w_out, row, col, need_ctx_out):
    (l_qkv, l_z, l_a, l_b, l_q, l_k, l_v, l_cb, l_cc, l_ch) = jnp.split(hl @ w_in, SPLIT_IDX, axis=-1)
    (c_qkv, c_z, c_a, c_b, c_q, c_k, c_v, c_cb, c_cc, c_ch) = jnp.split(hc @ w_in, SPLIT_IDX, axis=-1)
    a_ctx = gdn_inputs(c_qkv, c_a, c_b, conv_a, a_log, dt_bias)
    a_lat = gdn_inputs(l_qkv, l_a, l_b, conv_a, a_log, dt_bias)
    oa_c, oa_l = gdn_bidirectional(a_ctx, a_lat)
    ya_l = gdn_output(oa_l, l_z, gdn_g)
    qc, kc, vc = attn_heads(c_q, c_k, c_v, q_n, k_n)
    ql, kl, vl = attn_heads(l_q, l_k, l_v, q_n, k_n)
    ql = axial_rope(ql, row, col)
    kl = axial_rope(kl, row, col)
    k_all = jnp.concatenate([kc, kl], axis=1)
    v_all = jnp.concatenate([vc, vl], axis=1)
    yb_l = block_attention(ql, k_all, v_all)
    yc_l = short_conv_mix(l_cb, l_cc, l_ch, conv_c)
    out_l = jnp.concatenate([ya_l, yb_l, yc_l], axis=-1) @ w_out
    if not need_ctx_out:
        return out_l, None
    ya_c = gdn_output(oa_c, c_z, gdn_g)
    yb_c = block_attention(qc, kc, vc)
    yc_c = short_conv_mix(c_cb, c_cc, c_ch, conv_c)
    out_c = jnp.concatenate([ya_c, yb_c, yc_c], axis=-1) @ w_out
    return out_l, out_c


def swiglu(h, wg, wu, wd):
    return (jax.nn.silu(h @ wg) * (h @ wu)) @ wd


def moe_swiglu(h, router, wg, wu, wd):
    probs = jax.nn.softmax((h @ router).astype(F32), axis=-1)
    top_p, top_i = lax.top_k(probs, TOP_K)
    top_p = top_p / jnp.sum(top_p, axis=-1, keepdims=True)
    comb = jnp.sum(jax.nn.one_hot(top_i, N_EXPERTS, dtype=F32) * top_p[..., None], axis=-2)
    comb = comb.astype(h.dtype)
    out = jnp.zeros_like(h)
    for e in range(N_EXPERTS):
        out = out + comb[..., e:e + 1] * swiglu(h, wg[e], wu[e], wd[e])
    return out


def channel_mixer(h, l, ffn_w_gate, ffn_w_up, ffn_w_down, router, moe_w_gate, moe_w_up, moe_w_down):
    i = l // 2
    if l % 2 == 0:
        return swiglu(h, ffn_w_gate[i], ffn_w_up[i], ffn_w_down[i])
    return moe_swiglu(h, router[i], moe_w_gate[i], moe_w_up[i], moe_w_down[i])


def setup_inputs(seed: int = 0) -> dict:
    key = jax.random.key(seed)
    ks = jax.random.split(key, 26)
    d = D_MODEL

    def nrm(k, shape, s):
        return jax.random.normal(k, shape, F32) * s

    dt = jnp.exp(jax.random.uniform(ks[11], (DEPTH, 2, GDN_HEADS), F32, math.log(1e-3), math.log(1e-1)))
    return {
        "x": nrm(ks[0], (BATCH, SEQ, d), 1.0),
        "c": nrm(ks[1], (BATCH, d), 1.0),
        "ctx": nrm(ks[2], (BATCH, CTX_LEN, d), 1.0),
        "c_ctx": nrm(ks[3], (d,), 1.0),
        "w_mod": nrm(ks[4], (DEPTH, d, 6 * d), 0.5 * d ** -0.5),
        "b_mod": nrm(ks[5], (DEPTH, 6 * d), 0.02),
        "norm1": 1.0 + nrm(ks[6], (DEPTH, d), 0.02),
        "norm2": 1.0 + nrm(ks[7], (DEPTH, d), 0.02),
        "w_in": nrm(ks[8], (DEPTH, d, IN_COLS), d ** -0.5),
        "conv_a": nrm(ks[9], (DEPTH, GDN_CONV, A_QKV), GDN_CONV ** -0.5),
        "a_log": jnp.log(jax.random.uniform(ks[10], (DEPTH, 2, GDN_HEADS), F32, 1.0, 16.0)),
        "dt_bias": dt + jnp.log(-jnp.expm1(-dt)),
        "gdn_norm": 1.0 + nrm(ks[12], (DEPTH, GDN_DV), 0.02),
        "q_norm": 1.0 + nrm(ks[13], (DEPTH, HEAD_DIM), 0.02),
        "k_norm": 1.0 + nrm(ks[14], (DEPTH, HEAD_DIM), 0.02),
        "conv_c": nrm(ks[15], (DEPTH, SC_CONV, SC_CH), SC_CONV ** -0.5),
        "w_out": nrm(ks[16], (DEPTH, MIX_WIDTH, d), MIX_WIDTH ** -0.5),
        "ffn_w_gate": nrm(ks[17], (N_DENSE, d, D_FF), d ** -0.5),
        "ffn_w_up": nrm(ks[18], (N_DENSE, d, D_FF), d ** -0.5),
        "ffn_w_down": nrm(ks[19], (N_DENSE, D_FF, d), D_FF ** -0.5),
        "router": nrm(ks[20], (N_MOE, d, N_EXPERTS), d ** -0.5),
        "moe_w_gate": nrm(ks[21], (N_MOE, N_EXPERTS, d, D_FF_EXPERT), d ** -0.5),
        "moe_w_up": nrm(ks[22], (N_MOE, N_EXPERTS, d, D_FF_EXPERT), d ** -0.5),
        "moe_w_down": nrm(ks[23], (N_MOE, N_EXPERTS, D_FF_EXPERT, d), D_FF_EXPERT ** -0.5),
        "norm_f": 1.0 + nrm(ks[24], (d,), 0.02),
    }


def reference(x, c, ctx, c_ctx, w_mod, b_mod, norm1, norm2, w_in, conv_a, a_log, dt_bias, gdn_norm,
              q_norm, k_norm, conv_c, w_out, ffn_w_gate, ffn_w_up, ffn_w_down, router,
              moe_w_gate, moe_w_up, moe_w_down, norm_f):
    row, col = grid_positions(x.shape[1])
    s_lat = jax.nn.silu(c)
    s_ctx = jax.nn.silu(c_ctx)
    hc_stream = ctx
    for l in range(DEPTH):
        last = l == DEPTH - 1
        m_lat = (s_lat @ w_mod[l] + b_mod[l])[:, None, :]
        m_ctx = s_ctx @ w_mod[l] + b_mod[l]
        sh1, sc1, g1, sh2, sc2, g2 = jnp.split(m_lat, 6, axis=-1)
        csh1, csc1, cg1, csh2, csc2, cg2 = jnp.split(m_ctx, 6, axis=-1)
        hl = modulate(rmsnorm(x, norm1[l]), sh1, sc1)
        hc = modulate(rmsnorm(hc_stream, norm1[l]), csh1, csc1)
        out_l, out_c = token_mixers(hl, hc, w_in[l], conv_a[l], a_log[l], dt_bias[l], gdn_norm[l],
                                    q_norm[l], k_norm[l], conv_c[l], w_out[l], row, col, not last)
        x = x + g1 * out_l
        hl2 = modulate(rmsnorm(x, norm2[l]), sh2, sc2)
        x = x + g2 * channel_mixer(hl2, l, ffn_w_gate, ffn_w_up, ffn_w_down, router,
                                   moe_w_gate, moe_w_up, moe_w_down)
        if not last:
            hc_stream = hc_stream + cg1 * out_c
            hc2 = modulate(rmsnorm(hc_stream, norm2[l]), csh2, csc2)
            hc_stream = hc_stream + cg2 * channel_mixer(hc2, l, ffn_w_gate, ffn_w_up, ffn_w_down, router,
                                                        moe_w_gate, moe_w_up, moe_w_down)
    return rmsnorm(x, norm_f)
```

```python
import numpy as np
import concourse.bass as bass
import concourse.mybir as mybir
from contextlib import ExitStack

F32 = mybir.dt.float32
BF16 = mybir.dt.bfloat16
AF = mybir.ActivationFunctionType
ALU = mybir.AluOpType
AX = mybir.AxisListType

ENGS = ("pe", "act", "dve", "pool", "sp")
KROT = 4


class Res:
    __slots__ = ("name", "w", "rd")

    def __init__(self, name=""):
        self.name = name
        self.w = None
        self.rd = []


class Ins:
    __slots__ = ("eng", "fn", "deps", "dma", "tok", "marked", "idx", "ph")

    def __init__(self, eng, fn, dma):
        self.eng = eng
        self.fn = fn
        self.deps = []
        self.dma = dma
        self.tok = None
        self.marked = False


class Buf:
    def __init__(self, t, res):
        self.t = t
        self.res = res

    def __getitem__(self, k):
        return self.t[k]


class Prog:
    def __init__(self, nc):
        self.nc = nc
        self.q = {e: [] for e in ENGS}
        self.es = ExitStack()
        self.dma_sems = {}
        self.dma_cnt = {}
        self.nbuf = 0
        self.dram_res = {}

    def sb(self, name, shape, dtype):
        self.nbuf += 1
        name = f"{name}_u{self.nbuf}"
        t = self.es.enter_context(self.nc.sbuf_tensor(name, list(shape), dtype))
        return Buf(t, Res(name))

    def ps(self, name, shape, dtype=F32):
        self.nbuf += 1
        name = f"{name}_u{self.nbuf}"
        t = self.es.enter_context(self.nc.psum_tensor(name, list(shape), dtype))
        return Buf(t, Res(name))

    def dram(self, name, shape, dtype, kind=None):
        if kind is None:
            t = self.nc.dram_tensor(name, list(shape), dtype)
        else:
            t = self.nc.dram_tensor(name, list(shape), dtype, kind=kind)
        return t.ap()

    def R(self, key):
        r = self.dram_res.get(key)
        if r is None:
            r = Res(str(key))
            self.dram_res[key] = r
        return r

    def add(self, eng, fn, reads=(), writes=(), dma=None):
        if dma is not None:
            km = self.__dict__.setdefault("keymap", {})
            dma = ("kp", km.setdefault(dma, len(km)))
        I = Ins(eng, fn, dma)
        I.ph = getattr(self, "phase", 0)
        deps = []
        for r in reads:
            r = r.res if isinstance(r, Buf) else r
            if r.w is not None:
                deps.append(r.w)
        for w in writes:
            w = w.res if isinstance(w, Buf) else w
            deps.extend(w.rd)
            if w.w is not None:
                deps.append(w.w)
        for d in deps:
            if d is I or d.ph != I.ph:
                continue
            if d.eng == "pe" and eng == "pe" and d.dma is None and dma is None:
                continue
            I.deps.append(d)
            d.marked = True
        for r in reads:
            r = r.res if isinstance(r, Buf) else r
            r.rd.append(I)
        for w in writes:
            w = w.res if isinstance(w, Buf) else w
            w.w = I
            w.rd = []
        self.q[eng].append(I)
        return I

    def dma(self, eng, out, in_, reads, writes, key, **kw):
        return self.add(eng, lambda e: e.dma_start(out=out, in_=in_, **kw), reads, writes, dma=key)

    def emit(self):
        nc = self.nc
        es = self.es
        esem = {e: [es.enter_context(nc.semaphore(f"s_{e}{k}")) for k in range(KROT)] for e in ENGS}
        for e in ENGS:
            m = 0
            for I in self.q[e]:
                if I.dma is not None:
                    key = I.dma
                    if key not in self.dma_sems:
                        self.dma_sems[key] = es.enter_context(nc.semaphore(f"d_{len(self.dma_sems)}"))
                        self.dma_cnt[key] = 0
                    self.dma_cnt[key] += 1
                    I.tok = (self.dma_sems[key], 16 * self.dma_cnt[key])
                elif I.marked:
                    I.tok = (esem[e][m % KROT], m // KROT + 1)
                    m += 1
        block = es.enter_context(nc.Block())
        stats = {}

        def run(eng_name, eh):
            waited = {}
            nw = 0
            for I in self.q[eng_name]:
                need = {}
                for d in I.deps:
                    s, v = d.tok
                    k = id(s)
                    if waited.get(k, 0) >= v:
                        continue
                    if k not in need or need[k][1] < v:
                        need[k] = (s, v)
                for k, (s, v) in need.items():
                    eh.wait_ge(s, v)
                    waited[k] = v
                    nw += 1
                h = I.fn(eh)
                if I.tok is not None:
                    if I.dma is not None:
                        h.then_inc(I.tok[0], 16)
                    else:
                        h.then_inc(I.tok[0], 1)
            stats[eng_name] = (len(self.q[eng_name]), nw)

        @block.tensor
        def _(e):
            run("pe", e)

        @block.scalar
        def _(e):
            run("act", e)

        @block.vector
        def _(e):
            run("dve", e)

        @block.gpsimd
        def _(e):
            run("pool", e)

        @block.sync
        def _(e):
            run("sp", e)

        self.stats = stats
        return stats

    def finish_wait(self, eng, instrs):
        I = Ins(eng, lambda e: e.nop(), None)
        for d in instrs:
            I.deps.append(d)
            d.marked = True
        self.q[eng].append(I)
        return I


def _phase_begin(self):
    self.pes = ExitStack()
    self._glob_es = self.es
    self.es = self.pes
    self.q = {e: [] for e in ENGS}
    self.phase = getattr(self, "phase", 0) + 1
    self.keymap = {}


def _phase_end(self):
    lasts = []
    for e in ENGS:
        for I in reversed(self.q[e]):
            if I.dma is None:
                lasts.append(I)
                break
    dmas = [I for e in ENGS for I in self.q[e] if I.dma is not None]
    for e in ENGS:
        self.finish_wait(e, lasts + dmas)
    self._emit_block()
    self.es = self._glob_es
    self.pes.close()
    for r in self.dram_res.values():
        r.w = None
        r.rd = []


def _emit_block(self):
    nc = self.nc
    ges = self._glob_es
    if not hasattr(self, "esem"):
        self.esem = {e: [ges.enter_context(nc.semaphore(f"s_{e}{k}")) for k in range(KROT)] for e in ENGS}
        self.ecnt = {e: 0 for e in ENGS}
        self.waited = {e: {} for e in ENGS}
        self.tot = {e: [0, 0] for e in ENGS}
    for e in ENGS:
        for I in self.q[e]:
            if I.dma is not None:
                key = I.dma
                if key not in self.dma_sems:
                    self.dma_sems[key] = ges.enter_context(nc.semaphore(f"d_{len(self.dma_sems)}"))
                    self.dma_cnt[key] = 0
                self.dma_cnt[key] += 1
                I.tok = (self.dma_sems[key], 16 * self.dma_cnt[key])
            elif I.marked:
                m = self.ecnt[e]
                I.tok = (self.esem[e][m % KROT], m // KROT + 1)
                self.ecnt[e] = m + 1
    with nc.Block() as block:
        def run(eng_name, eh):
            waited = self.waited[eng_name]
            for I in self.q[eng_name]:
                need = {}
                for d in I.deps:
                    s, v = d.tok
                    k = id(s)
                    if waited.get(k, 0) >= v:
                        continue
                    if k not in need or need[k][1] < v:
                        need[k] = (s, v)
                for k, (s, v) in need.items():
                    eh.wait_ge(s, v)
                    waited[k] = v
                    self.tot[eng_name][1] += 1
                h = I.fn(eh)
                self.tot[eng_name][0] += 1
                if I.tok is not None:
                    h.then_inc(I.tok[0], 16 if I.dma is not None else 1)

        @block.tensor
        def _(e):
            run("pe", e)

        @block.scalar
        def _(e):
            run("act", e)

        @block.vector
        def _(e):
            run("dve", e)

        @block.gpsimd
        def _(e):
            run("pool", e)

        @block.sync
        def _(e):
            run("sp", e)


Prog.phase_begin = _phase_begin
Prog.phase_end = _phase_end
Prog._emit_block = _emit_block
from concourse.bass_utils import run_bass_kernel_spmd

D = 1024
L = 4096
CT = 256
T = L + CT
TP = T + 4
NL = 4
DFF = 2816
DFE = 1408
NE = 8
EPS = 1e-6
NFM = 24
NTM = 400
NCOLS = NFM * 128 + NTM
R_N1, R_N2, R_BM, R_CA, R_CC, R_QG, R_QGP, R_KG, R_KGP, R_NF, NVR = 0, 8, 16, 64, 82, 88, 89, 90, 91, 92, 100
CHK = 128
NOCAST = False
GDN_STEPS = None
GSUB = 99
IMPLEMENTED_FFN = True
SUB = 9
C0SHIFT = 0
SEC = 9
NTILES = 99


def pc(t):
    return t + 1 if t < CT else t + 3


def tiles_of_batch():
    return [(0, CT)] + [(CT + 512 * i, 512) for i in range(L // 512)]


class M:
    pass


def build(nlayers=NL, debug=(), stop=None, skip=()):
    nc = bass.Bass("TRN2", target_bir_lowering=False)
    P = Prog(nc)
    m = M()
    m.nc, m.P = nc, P
    m.nocast = NOCAST
    dbg = set(debug)

    def scratch(name, shape, dt):
        return P.dram(name, shape, dt, kind="ExternalOutput" if name in dbg else None)

    I_ = lambda n, s, dt=F32: P.dram(n, s, dt, kind="ExternalInput")
    m.x_in = I_("x", [2, L, D])
    m.ctx_in = I_("ctx", [2, CT, D])
    m.cvec = I_("cvec", [4, D])
    m.w_mod = I_("w_mod", [NL, D, 6 * D])
    m.vecs = I_("vecs", [NL, NVR, 128])
    m.gdn_g = I_("gdn_g", [NL, 64])
    m.gatec = I_("gatec", [NL, 16])
    m.w_in = I_("w_in", [NL, D, NCOLS])
    m.w_out = I_("w_out", [NL, D, D])
    m.ffn_g = I_("ffn_g", [2, D, DFF])
    m.ffn_u = I_("ffn_u", [2, D, DFF])
    m.ffn_d = I_("ffn_d", [2, DFF, D])
    m.router = I_("router", [2, D, NE])
    m.moe_g = I_("moe_g", [2, NE, D, DFE])
    m.moe_u = I_("moe_u", [2, NE, D, DFE])
    m.moe_d = I_("moe_d", [2, NE, DFE, D])
    m.ropeC = I_("ropeC", [128, T])
    m.ropeS = I_("ropeS", [128, T])
    m.gmask_in = I_("gmask", [128, 7, 128])
    m.out = P.dram("out", [2, L, D], F32, kind="ExternalOutput")
    m.w_in_b = P.dram("w_in_b", [NL, D, NCOLS], BF16)
    m.w_out_b = P.dram("w_out_b", [NL, D, D], BF16)
    m.ffn_g_b = P.dram("ffn_g_b", [2, D, DFF], BF16)
    m.ffn_u_b = P.dram("ffn_u_b", [2, D, DFF], BF16)
    m.ffn_d_b = P.dram("ffn_d_b", [2, DFF, D], BF16)
    m.moe_g_b = P.dram("moe_g_b", [2, NE, D, DFE], BF16)
    m.moe_u_b = P.dram("moe_u_b", [2, NE, D, DFE], BF16)
    m.moe_d_b = P.dram("moe_d_b", [2, NE, DFE, D], BF16)
    m.XT = scratch("XT", [2, D, T], F32)
    m.QKVp = scratch("QKVp", [2, 768, TP], F32)
    m.Zs = scratch("Zs", [2, T, 256], F32)
    m.G = scratch("G", [2, 16, T], F32)
    m.QA = scratch("QA", [2, 512, T], BF16)
    m.KA = scratch("KA", [2, 2, 128, T], BF16)
    m.VA = scratch("VA", [2, T, 128], BF16)
    m.CB = scratch("CB", [2, 256, T], F32)
    m.PP = scratch("PP", [2, 256, TP], F32)
    m.QG = scratch("QG", [2, 256, T], BF16)
    m.KG = scratch("KG", [2, 256, T], BF16)
    m.KGt = scratch("KGt", [2, T, 256], F32)
    m.VGt = scratch("VGt", [2, T, 256], F32)
    m.OG = scratch("OG", [2, 2, T, 256], F32)
    m.YT = scratch("YT", [2, D, T], BF16)
    m.debug = dbg

    c = M()
    m.c = c
    c.ident = P.sb("ident", [128, 128], F32)
    c.identb = P.sb("identb", [128, 128], BF16)
    c.onesb = P.sb("onesb", [128, 128], BF16)
    c.bdiag = P.sb("bdiag", [128, 128], BF16)
    c.onesf = P.sb("onesf", [128, 128], F32)
    c.vcol = P.sb("vcol", [128, NL, NVR], F32)
    c.modv = P.sb("modv", [128, NL, 48, 4], F32)
    c.gm1 = P.sb("gm1", [128, NL, 8, 4], F32)
    c.gm2 = P.sb("gm2", [128, NL, 8, 4], F32)
    c.gatec = P.sb("gatecs", [128, NL, 16], F32)
    c.gdng = P.sb("gdng", [128, NL, 64], F32)
    c.zero = P.sb("zero", [128, 512], F32)
    m.gcol = P.sb("gcol", [8, NL, 2], F32)
    c.sel = P.sb("sel", [8, 8, 128], F32)
    c.gmask = P.sb("gmaskc", [128, 7, 128], F32)
    c.Ui = P.sb("Ui", [128, 128], F32)
    c.Us = P.sb("Us", [128, 128], F32)
    c.Li = P.sb("Li", [128, 128], F32)
    c.Ls = P.sb("Ls", [128, 128], F32)
    c.zerob = P.sb("zerob", [128, 2176], BF16)
    m.skip = set(skip)

    P.phase_begin()
    prologue(m)
    P.phase_end()
    if stop == "pro":
        return finish(m)
    for l in range(nlayers):
        last = (l == NL - 1)
        P.phase_begin(); stage_proj(m, l); P.phase_end()
        if stop == ("s1", l): return finish(m)
        if 'gdn' in skip:
            P.phase_begin(); stage_attn(m, l, last); P.phase_end()
            if stop == ('at', l): return finish(m)
            P.phase_begin(); stage_ffn(m, l, last); P.phase_end()
            if stop == ('ff', l): return finish(m)
            continue
        P.phase_begin(); stage_gdn_pre(m, l); P.phase_end()
        if stop == ("g1", l): return finish(m)
        P.phase_begin(); stage_gdn_scan(m, l); P.phase_end()
        if stop == ("g2", l): return finish(m)
        P.phase_begin(); stage_gdn_fin(m, l, last); P.phase_end()
        if stop == ("g3", l): return finish(m)
        P.phase_begin(); stage_attn(m, l, last); P.phase_end()
        if stop == ("at", l): return finish(m)
        P.phase_begin(); stage_ffn(m, l, last); P.phase_end()
        if stop == ("ff", l): return finish(m)
    P.phase_begin(); epilogue(m); P.phase_end()
    return finish(m)


def finish(m):
    m.P._glob_es = m.P.es
    m.P.es.close()
    return m


def cast_dma(P, dst, src, key, rows):
    n = src.shape[0]
    for r0 in range(0, n, rows):
        r1 = min(n, r0 + rows)
        P.add("pool", (lambda o, i: (lambda e: e.dma_start(out=o, in_=i, max_dma_last_dim=4096)))(dst[r0:r1, :], src[r0:r1, :]),
              [], [P.R(key)], dma="cast")


def casts_proj(m, l):
    P = m.P
    cast_dma(P, m.w_in_b[l], m.w_in[l], ("w_in_b", l), 512)
    cast_dma(P, m.w_out_b[l], m.w_out[l], ("w_out_b", l), 512)


def casts_ffn(m, l):
    P = m.P
    i = l // 2
    if l % 2 == 0:
        cast_dma(P, m.ffn_g_b[i], m.ffn_g[i], ("ffn_g_b", i), 512)
        cast_dma(P, m.ffn_u_b[i], m.ffn_u[i], ("ffn_u_b", i), 512)
        cast_dma(P, m.ffn_d_b[i], m.ffn_d[i], ("ffn_d_b", i), 704)
    else:
        for e in range(NE):
            cast_dma(P, m.moe_g_b[i, e], m.moe_g[i, e], ("moe_g_b", i, e), 512)
            cast_dma(P, m.moe_u_b[i, e], m.moe_u[i, e], ("moe_u_b", i, e), 512)
            cast_dma(P, m.moe_d_b[i, e], m.moe_d[i, e], ("moe_d_b", i, e), 704)


def prologue(m):
    P, c = m.P, m.c
    P.add("pool", lambda e: e.memset(c.ident[:, :], 1.0), [], [c.ident])
    P.add("pool", lambda e: e.affine_select(out=c.ident[:, :], in_=c.ident[:, :], pattern=[[-1, 128]],
                                            compare_op=ALU.is_equal, fill=0.0, base=0, channel_multiplier=1),
          [c.ident], [c.ident])
    P.add("pool", lambda e: e.tensor_copy(out=c.identb[:, :], in_=c.ident[:, :]), [c.ident], [c.identb])
    P.add("pool", lambda e: e.memset(c.onesb[:, :], 1.0), [], [c.onesb])
    P.add("pool", lambda e: e.memset(c.onesf[:, :], 1.0), [], [c.onesf])
    P.add("pool", lambda e: e.memset(c.zero[:, :], 0.0), [], [c.zero])
    P.add("pool", lambda e: e.memset(c.bdiag[:, :], 0.0), [], [c.bdiag])
    P.add("pool", lambda e: e.memset(c.bdiag[0:64, 0:64], 1.0), [], [c.bdiag])
    P.add("pool", lambda e: e.memset(c.bdiag[64:128, 64:128], 1.0), [], [c.bdiag])
    P.add("pool", lambda e: e.tensor_copy(out=c.sel[:, :, :], in_=c.ident[0:8, 0:8].unsqueeze(2).to_broadcast([8, 8, 128])), [c.ident], [c.sel])
    for (mk, coef, cm, op) in ((c.Ui, 1, -1, ALU.is_ge), (c.Us, 1, -1, ALU.is_gt), (c.Li, -1, 1, ALU.is_ge), (c.Ls, -1, 1, ALU.is_gt)):
        P.add("pool", (lambda mk: (lambda e: e.memset(mk[:, :], 1.0)))(mk), [], [mk])
        P.add("pool", (lambda mk, coef, cm, op: (lambda e: e.affine_select(out=mk[:, :], in_=mk[:, :], pattern=[[coef, 128]], compare_op=op, fill=0.0, base=0, channel_multiplier=cm)))(mk, coef, cm, op), [mk], [mk])
    casts_proj(m, 0)
    if "gdn" in m.skip:
        P.add("pool", lambda e: e.memset(c.zerob[:, :], 0.0), [], [c.zerob])
        for b in range(2):
            for hf_ in range(2):
                for cc_ in range(2):
                    P.dma("sp", m.YT[b][cc_ * 128:(cc_ + 1) * 128, hf_ * 2176:(hf_ + 1) * 2176], c.zerob[:, :], [c.zerob], [P.R(("YTz", b, hf_, cc_))], "padz")
    for b in range(2):
        for (dst, nr) in ((m.QKVp, 768), (m.PP, 256)):
            v = dst[b].rearrange("(c p) t -> p c t", p=128)
            ncn = nr // 128
            for (a0, a1) in ((0, 1), (CT + 1, CT + 3), (TP - 1, TP)):
                P.dma("sp", v[:, :, a0:a1], c.zero[:, 0:ncn * (a1 - a0)].rearrange("p (c t) -> p c t", c=ncn),
                      [c.zero], [P.R(("pad", b, nr, a0))], "padz", allow_slow_non_contiguous=True)
    ps = [P.ps(f"pps{i}", [128, 512]) for i in range(4)]
    for l in range(NL):
        vt = P.sb(f"vt{l}", [NVR, 128], F32)
        P.dma("sp", vt[:, :], m.vecs[l], [], [vt], ("vt", l))
        P.add("pe", (lambda l, vt: (lambda e: e.transpose(out=ps[0][:, l * NVR:(l + 1) * NVR], in_=vt[:, :], identity=c.ident[0:NVR, 0:NVR])))(l, vt),
              [vt, c.ident], [ps[0]])
    P.add("dve", lambda e: e.tensor_copy(out=c.vcol[:, :, :], in_=ps[0][:, 0:NL * NVR].rearrange("p (l r) -> p l r", l=NL)), [ps[0]], [c.vcol])
    P.dma("sp", c.gmask[:, :, :], m.gmask_in, [], [c.gmask], "gmaskc")
    P.dma("sp", c.gatec[:, :, :], m.gatec.partition_broadcast(128), [], [c.gatec], "gatec")
    P.dma("sp", c.gdng[:, :, :], m.gdn_g.partition_broadcast(128), [], [c.gdng], "gdng")
    P.dma("sp", m.gcol[:, :, :], m.gatec.rearrange("l (j p) -> p l j", p=8), [], [m.gcol], "gcol", allow_slow_non_contiguous=True)
    P.add("act", lambda e: e.activation(out=m.gcol[:, :, 1:2], in_=m.gcol[:, :, 1:2], func=AF.Exp), [m.gcol], [m.gcol])
    P.add("dve", lambda e: e.tensor_scalar(out=m.gcol[:, :, 1:2], in0=m.gcol[:, :, 1:2], scalar1=-1.0, scalar2=None, op0=ALU.mult), [m.gcol], [m.gcol])
    P.add("act", lambda e: e.activation(out=c.gatec[:, :, 8:16], in_=c.gatec[:, :, 8:16], func=AF.Exp), [c.gatec], [c.gatec])
    P.add("dve", lambda e: e.tensor_scalar(out=c.gatec[:, :, 8:16], in0=c.gatec[:, :, 8:16], scalar1=-1.0, scalar2=None, op0=ALU.mult), [c.gatec], [c.gatec])
    cv = P.sb("cv", [4, D], F32)
    P.dma("sp", cv[:, :], m.cvec, [], [cv], "cv")
    P.add("act", lambda e: e.activation(out=cv[:, :], in_=cv[:, :], func=AF.Silu), [cv], [cv])
    sT = P.sb("sT", [128, 8, 4], F32)
    for kc in range(8):
        P.add("pe", (lambda kc: (lambda e: e.transpose(out=ps[1][:, kc * 4:kc * 4 + 4], in_=cv[:, kc * 128:(kc + 1) * 128], identity=c.ident[0:4, 0:4])))(kc),
              [cv, c.ident], [ps[1]])
    P.add("dve", lambda e: e.tensor_copy(out=sT[:, :, :], in_=ps[1][:, 0:32].rearrange("p (k w) -> p k w", k=8)), [ps[1]], [sT])
    wm = [P.sb(f"wm{i}", [128, 8, 512], F32) for i in range(2)]
    it = 0
    for l in range(NL):
        pm = ps[2 + (l % 2)]
        for g in range(12):
            w = wm[it % 2]
            it += 1
            P.dma("sp", w[:, :, :], m.w_mod[l][:, g * 512:(g + 1) * 512].rearrange("(k p) n -> p k n", p=128), [], [w], ("wm", it % 2))
            for j4 in range(4):
                j = g * 4 + j4
                for kc in range(8):
                    P.add("pe", (lambda w, j, j4, kc, pm: (lambda e: e.matmul(pm[:, j * 4:j * 4 + 4], lhsT=w[:, kc, j4 * 128:(j4 + 1) * 128], rhs=sT[:, kc, :],
                                                                               start=(kc == 0), stop=(kc == 7))))(w, j, j4, kc, pm),
                          [w, sT], [pm])
        P.add("dve", (lambda l, pm: (lambda e: e.tensor_tensor(out=c.modv[:, l, :, :], in0=pm[:, 0:192].rearrange("p (j w) -> p j w", w=4),
                                                                in1=c.vcol[:, l, R_BM:R_BM + 48].unsqueeze(2).to_broadcast([128, 48, 4]), op=ALU.add)))(l, pm),
              [pm, c.vcol], [c.modv])
        P.add("dve", (lambda l: (lambda e: e.scalar_tensor_tensor(out=c.gm1[:, l, :, :], in0=c.modv[:, l, 8:16, :], scalar=1.0,
                                                                  in1=c.vcol[:, l, R_N1:R_N1 + 8].unsqueeze(2).to_broadcast([128, 8, 4]), op0=ALU.add, op1=ALU.mult)))(l),
              [c.modv, c.vcol], [c.gm1])
        P.add("dve", (lambda l: (lambda e: e.scalar_tensor_tensor(out=c.gm2[:, l, :, :], in0=c.modv[:, l, 32:40, :], scalar=1.0,
                                                                  in1=c.vcol[:, l, R_N2:R_N2 + 8].unsqueeze(2).to_broadcast([128, 8, 4]), op0=ALU.add, op1=ALU.mult)))(l),
              [c.modv, c.vcol], [c.gm2])
    xin = [P.sb(f"xin{i}", [128, 4, D], F32) for i in range(2)]
    xo = [P.sb(f"xo{i}", [128, 8, 512], F32) for i in range(2)]
    pt = [P.ps(f"ppt{i}", [128, 512]) for i in range(4)]
    it = 0
    for b in range(2):
        for (t0, Tt) in tiles_of_batch():
            nb = Tt // 128
            xi, xout = xin[it % 2], xo[it % 2]
            it += 1
            src = m.ctx_in[b] if t0 < CT else m.x_in[b, t0 - CT:t0 - CT + Tt, :]
            P.dma("sp", xi[:, 0:nb, :], src.rearrange("(n p) d -> p n d", p=128), [], [xi], ("xin", it % 2))
            for cc in range(8):
                pp = pt[cc % 4]
                for n in range(nb):
                    P.add("pe", (lambda pp, xi, n, cc: (lambda e: e.transpose(out=pp[:, n * 128:(n + 1) * 128], in_=xi[:, n, cc * 128:(cc + 1) * 128], identity=c.ident[:, :])))(pp, xi, n, cc),
                          [xi, c.ident], [pp])
                eng = "dve" if cc % 2 == 0 else "act"
                if eng == "dve":
                    P.add("dve", (lambda pp, xout, cc, Tt: (lambda e: e.tensor_copy(out=xout[:, cc, 0:Tt], in_=pp[:, 0:Tt])))(pp, xout, cc, Tt), [pp], [xout])
                else:
                    P.add("act", (lambda pp, xout, cc, Tt: (lambda e: e.activation(out=xout[:, cc, 0:Tt], in_=pp[:, 0:Tt], func=AF.Copy)))(pp, xout, cc, Tt), [pp], [xout])
            P.dma("sp", m.XT[b].rearrange("(c p) t -> p c t", p=128)[:, :, t0:t0 + Tt], xout[:, :, 0:Tt], [xout], [P.R(("XT", b, t0))], ("xo", it % 2))


def _perm64():
    p = np.zeros(64, np.int64)
    for d in range(64):
        p[d] = d + 16 if (d % 32) < 16 else d - 16
    return p


def host_prep(inp):
    f = lambda a: np.ascontiguousarray(np.asarray(a, dtype=np.float32))
    w_in = f(inp["w_in"])
    p64 = _perm64()
    o_qkv, o_z, o_a, o_b, o_q, o_k, o_v, o_cb, o_cc, o_ch = 0, 768, 1024, 1032, 1040, 1552, 1680, 1808, 2064, 2320
    cols = []
    cols += list(range(o_qkv, o_qkv + 768))
    cols += list(range(o_q, o_q + 512))
    for h in range(8):
        cols += list(o_q + h * 64 + p64)
    k0 = list(range(o_k, o_k + 64)); k1 = list(range(o_k + 64, o_k + 128))
    k0p = list(o_k + p64); k1p = list(o_k + 64 + p64)
    cols += k0 + k1 + k1 + k0 + k0p + k1p + k1p + k0p
    cols += list(range(o_cb, o_cb + 768))
    cols += list(range(o_z, o_z + 256)) + list(range(o_a, o_a + 16)) + list(range(o_v, o_v + 128))
    cols = np.array(cols)
    assert cols.size == NCOLS
    w_in_ext = np.ascontiguousarray(w_in[:, :, cols])
    vecs = np.zeros((NL, NVR, 128), np.float32)
    qn, kn = f(inp["q_norm"]), f(inp["k_norm"])
    for l in range(NL):
        vecs[l, R_N1:R_N1 + 8] = f(inp["norm1"])[l].reshape(8, 128)
        vecs[l, R_N2:R_N2 + 8] = f(inp["norm2"])[l].reshape(8, 128)
        vecs[l, R_BM:R_BM + 48] = f(inp["b_mod"])[l].reshape(48, 128)
        vecs[l, R_CA:R_CA + 18] = f(inp["conv_a"])[l].reshape(18, 128)
        vecs[l, R_CC:R_CC + 6] = f(inp["conv_c"])[l].reshape(6, 128)
        vecs[l, R_QG] = np.tile(qn[l], 2)
        vecs[l, R_QGP] = np.tile(qn[l][p64], 2)
        vecs[l, R_KG] = np.tile(kn[l], 2)
        vecs[l, R_KGP] = np.tile(kn[l][p64], 2)
        vecs[l, R_NF:R_NF + 8] = f(inp["norm_f"]).reshape(8, 128)
    gatec = np.concatenate([f(inp["dt_bias"]).reshape(NL, 8), f(inp["a_log"]).reshape(NL, 8)], axis=1)
    quarter = 16
    inv_freq = (10000.0 ** (-np.arange(quarter, dtype=np.float32) / quarter)).astype(np.float32)
    t = np.arange(L)
    row, col = (t // 64).astype(np.float32), (t % 64).astype(np.float32)
    C = np.ones((64, T), np.float32); S = np.zeros((64, T), np.float32)
    for d in range(64):
        pos = row if d < 32 else col
        fi = d % 16
        ang = (pos * inv_freq[fi]).astype(np.float32)
        C[d, CT:] = np.cos(ang)
        S[d, CT:] = np.sin(ang) * (-1.0 if (d % 32) < 16 else 1.0)
    ropeC = np.ascontiguousarray(np.concatenate([C, C], 0)); ropeS = np.ascontiguousarray(np.concatenate([S, S], 0))
    idx = np.arange(128)
    gmask = np.zeros((128, 7, 128), np.float32)
    md = lambda sz: (idx[:, None] // sz == idx[None, :] // sz).astype(np.float32)
    gmask[:, 0, :] = md(2)
    for k_, sz in enumerate((2, 4, 8, 16, 32, 64)):
        gmask[:, 1 + k_, :] = md(2 * sz) - md(sz)
    shared = dict(gmask=gmask, w_mod=f(inp["w_mod"]), vecs=vecs, gdn_g=f(inp["gdn_norm"]), gatec=np.ascontiguousarray(gatec), w_in=w_in_ext,
                  w_out=f(inp["w_out"]), ffn_g=f(inp["ffn_w_gate"]), ffn_u=f(inp["ffn_w_up"]), ffn_d=f(inp["ffn_w_down"]),
                  router=f(inp["router"]), moe_g=f(inp["moe_w_gate"]), moe_u=f(inp["moe_w_up"]), moe_d=f(inp["moe_w_down"]),
                  ropeC=ropeC, ropeS=ropeS)
    x, ctx, cc, c_ctx = f(inp["x"]), f(inp["ctx"]), f(inp["c"]), f(inp["c_ctx"])
    maps = []
    for core in range(8):
        b0 = 2 * core
        cvec = np.zeros((4, D), np.float32)
        cvec[0], cvec[1], cvec[2] = cc[b0], cc[b0 + 1], c_ctx
        d = dict(shared)
        d.update(x=np.ascontiguousarray(x[b0:b0 + 2]), ctx=np.ascontiguousarray(ctx[b0:b0 + 2]), cvec=cvec)
        maps.append(d)
    return maps


_CACHE = {}


def kernel(**inputs):
    maps = host_prep(inputs)
    if "m" not in _CACHE:
        _CACHE["m"] = build()
    m = _CACHE["m"]
    res = run_bass_kernel_spmd(m.nc, maps, core_ids=list(range(8)))
    return np.concatenate([r["out"] for r in res.results], axis=0)


def norm_mod(m, l, which, xt, sq, hT, pss, Tt, gm, sh_j0, hf=None):
    P, c = m.P, m.c
    P.add("act", lambda e: e.activation(out=sq[:, :, 0:Tt], in_=xt[:, :, 0:Tt], func=AF.Square), [xt], [sq])
    for kc in range(8):
        P.add("pe", (lambda kc: (lambda e: e.matmul(pss[:, 0:Tt], lhsT=c.onesb[:, :], rhs=sq[:, kc, 0:Tt], start=(kc == 0), stop=(kc == 7))))(kc), [sq, c.onesb], [pss])
    rs = m.rs
    P.add("act", lambda e: e.activation(out=rs[:, 0:Tt], in_=pss[:, 0:Tt], func=AF.Sqrt, scale=1.0 / D, bias=m.epsc[:, 0:1]), [pss, m.epsc], [rs])
    P.add("dve", lambda e: e.reciprocal(out=rs[:, 0:Tt], in_=rs[:, 0:Tt]), [rs], [rs])
    tmp = m.nm_tmp
    for kc in range(8):
        P.add("dve", (lambda kc: (lambda e: e.scalar_tensor_tensor(out=tmp[kc % 2][:, 0:Tt], in0=xt[:, kc, 0:Tt], scalar=gm[:, l, kc, which:which + 1], in1=rs[:, 0:Tt],
                                                                   op0=ALU.mult, op1=ALU.mult)))(kc), [xt, rs], [tmp[kc % 2]])
        dst = hT if hf is None else hf
        P.add("act", (lambda kc, dst: (lambda e: e.activation(out=dst[:, kc, 0:Tt], in_=tmp[kc % 2][:, 0:Tt], func=AF.Identity,
                                                              bias=c.modv[:, l, sh_j0 + kc, which:which + 1], scale=1.0)))(kc, dst), [tmp[kc % 2], c.modv], [dst])
    if hf is not None:
        P.add("pool", lambda e: e.tensor_copy(out=hT[:, :, 0:Tt], in_=hf[:, :, 0:Tt]), [hf], [hT])


def alloc_norm(m):
    P = m.P
    m.rs = P.sb("rs", [128, 512], F32)
    m.nm_tmp = [P.sb(f"nmt{i}", [128, 512], F32) for i in range(2)]
    m.epsc = P.sb("epsc", [128, 1], F32)
    P.add("pool", lambda e: e.memset(m.epsc[:, :], EPS), [], [m.epsc])


def stage_proj(m, l):
    P, c = m.P, m.c
    if not getattr(m, "nocast", False):
        if IMPLEMENTED_FFN:
            casts_ffn(m, l)
        if l + 1 < NL:
            casts_proj(m, l + 1)
    alloc_norm(m)
    st_v = P.sb("st_v", [128, 4, 128], BF16)
    W = P.sb("Win", [128, 8, NCOLS], BF16)
    for kc in range(8):
        P.dma("sp", W[:, kc, :], m.w_in_b[l][kc * 128:(kc + 1) * 128, :], [P.R(("w_in_b", l))], [W], "Win")
    xts = [P.sb(f"xt{i}", [128, 8, 512], F32) for i in range(2)]
    tabs = [P.sb(f"tab{i}", [128, 2, 512], F32) for i in range(2)]
    sq = P.sb("sq", [128, 8, 512], BF16)
    hT = P.sb("hT", [128, 8, 512], BF16)
    st_qkv = P.sb("st_qkv", [128, 6, 512], F32)
    st_q = P.sb("st_q", [128, 4, 512], BF16)
    st_k = P.sb("st_k", [128, 2, 512], BF16)
    st_cb = P.sb("st_cb", [128, 2, 512], F32)
    st_pp = P.sb("st_pp", [128, 2, 512], F32)
    st_z = P.sb("st_z", [128, 4, 256], F32)
    st_g = P.sb("st_g", [8, 2, 512], F32)
    zT = P.sb("zT", [128, 3, 512], F32)
    gt = [P.sb(f"gt{i}", [8, 512], F32) for i in range(3)]
    sqq = P.sb("sqq", [128, 512], BF16)
    rq = P.sb("rq", [128, 512], F32)
    t1 = P.sb("t1", [128, 512], F32)
    t2 = P.sb("t2", [128, 512], F32)
    ccs = P.sb("ccs", [128, 512], F32)
    ps = [P.ps(f"ps{i}", [128, 512]) for i in range(8)]
    it = 0
    tl = [(b, t0, Tt) for b in range(2) for (t0, Tt) in tiles_of_batch()]

    def load(i):
        b, t0, Tt = tl[i]
        P.dma("sp", xts[i % 2][:, :, 0:Tt], m.XT[b].rearrange("(c p) t -> p c t", p=128)[:, :, t0:t0 + Tt], [P.R(("XT", b, t0))], [xts[i % 2]], ("ldx", i % 2))
        P.dma("sp", tabs[i % 2][:, 0, 0:Tt], m.ropeC[:, t0:t0 + Tt], [], [tabs[i % 2]], ("ldt", i % 2))
        P.dma("sp", tabs[i % 2][:, 1, 0:Tt], m.ropeS[:, t0:t0 + Tt], [], [tabs[i % 2]], ("ldt", i % 2))

    def body(i, b, t0, Tt):
        xt, tab = xts[i % 2], tabs[i % 2]
        which = 2 if t0 < CT else b
        norm_mod(m, l, which, xt, sq, hT, ps[7], Tt, c.gm1, 0)
        nsub = Tt // 128

        def fm(j, pst):
            for kc in range(8):
                P.add("pe", (lambda kc: (lambda e: e.matmul(pst[:, 0:Tt], lhsT=W[:, kc, j * 128:(j + 1) * 128], rhs=hT[:, kc, 0:Tt], start=(kc == 0), stop=(kc == 7))))(kc), [W, hT], [pst])

        for j in range(6):
            pst = ps[j % 2]
            fm(j, pst)
            if j % 2 == 0:
                P.add("dve", (lambda j, pst: (lambda e: e.tensor_copy(out=st_qkv[:, j, 0:Tt], in_=pst[:, 0:Tt])))(j, pst), [pst], [st_qkv])
            else:
                P.add("act", (lambda j, pst: (lambda e: e.activation(out=st_qkv[:, j, 0:Tt], in_=pst[:, 0:Tt], func=AF.Copy)))(j, pst), [pst], [st_qkv])
        P.dma("sp", m.QKVp[b].rearrange("(c p) t -> p c t", p=128)[:, :, pc(t0):pc(t0) + Tt], st_qkv[:, :, 0:Tt], [st_qkv], [P.R(("QKVp", b, t0))], "st_qkv")

        if SEC < 2: return
        qkc = [0]

        def qk_chunk(jq, jp, rg, rgp, dst, stb):
            pq, pp_ = (ps[2], ps[3]) if qkc[0] % 2 == 0 else (ps[5], ps[6])
            qkc[0] += 1
            fm(jq, pq)
            fm(jp, pp_)
            P.add("act", lambda e: e.activation(out=sqq[:, 0:Tt], in_=pq[:, 0:Tt], func=AF.Square), [pq], [sqq])
            P.add("pe", lambda e: e.matmul(ps[4][:, 0:Tt], lhsT=c.bdiag[:, :], rhs=sqq[:, 0:Tt], start=True, stop=True), [sqq, c.bdiag], [ps[4]])
            P.add("act", lambda e: e.activation(out=rq[:, 0:Tt], in_=ps[4][:, 0:Tt], func=AF.Sqrt, scale=1.0 / 64, bias=m.epsc[:, 0:1]), [ps[4], m.epsc], [rq])
            P.add("dve", lambda e: e.reciprocal(out=rq[:, 0:Tt], in_=rq[:, 0:Tt]), [rq], [rq])
            P.add("dve", lambda e: e.scalar_tensor_tensor(out=t1[:, 0:Tt], in0=pq[:, 0:Tt], scalar=c.vcol[:, l, rg:rg + 1], in1=tab[:, 0, 0:Tt], op0=ALU.mult, op1=ALU.mult), [pq, tab, c.vcol], [t1])
            P.add("dve", lambda e: e.scalar_tensor_tensor(out=t2[:, 0:Tt], in0=pp_[:, 0:Tt], scalar=c.vcol[:, l, rgp:rgp + 1], in1=tab[:, 1, 0:Tt], op0=ALU.mult, op1=ALU.mult), [pp_, tab, c.vcol], [t2])
            P.add("pool", lambda e: e.tensor_tensor(out=t1[:, 0:Tt], in0=t1[:, 0:Tt], in1=t2[:, 0:Tt], op=ALU.add), [t1, t2], [t1])
            P.add("pool", lambda e: e.tensor_tensor(out=dst, in0=t1[:, 0:Tt], in1=rq[:, 0:Tt], op=ALU.mult), [t1, rq], [stb])

        for jq in range(4):
            qk_chunk(6 + jq, 10 + jq, R_QG, R_QGP, st_q[:, jq, 0:Tt], st_q)
        P.dma("sp", m.QA[b].rearrange("(c p) t -> p c t", p=128)[:, :, t0:t0 + Tt], st_q[:, :, 0:Tt], [st_q], [P.R(("QA", b, t0))], "st_q")
        for jk in range(2):
            qk_chunk(14 + jk, 16 + jk, R_KG, R_KGP, st_k[:, jk, 0:Tt], st_k)
        P.dma("sp", m.KA[b].rearrange("s p t -> p s t")[:, :, t0:t0 + Tt], st_k[:, :, 0:Tt], [st_k], [P.R(("KA", b, t0))], "st_k")

        if SEC < 3: return
        for j in range(2):
            fm(18 + j, ps[j % 2])
            P.add("act", (lambda j: (lambda e: e.activation(out=st_cb[:, j, 0:Tt], in_=ps[j % 2][:, 0:Tt], func=AF.Copy)))(j), [ps[j % 2]], [st_cb])
        P.dma("sp", m.CB[b].rearrange("(c p) t -> p c t", p=128)[:, :, t0:t0 + Tt], st_cb[:, :, 0:Tt], [st_cb], [P.R(("CB", b, t0))], "st_cb")
        for j in range(2):
            fm(20 + j, ps[2])
            fm(22 + j, ps[3])
            P.add("act", lambda e: e.activation(out=ccs[:, 0:Tt], in_=ps[2][:, 0:Tt], func=AF.Copy), [ps[2]], [ccs])
            P.add("dve", (lambda j: (lambda e: e.tensor_tensor(out=st_pp[:, j, 0:Tt], in0=ps[3][:, 0:Tt], in1=ccs[:, 0:Tt], op=ALU.mult)))(j), [ps[3], ccs], [st_pp])
        P.dma("sp", m.PP[b].rearrange("(c p) t -> p c t", p=128)[:, :, pc(t0):pc(t0) + Tt], st_pp[:, :, 0:Tt], [st_pp], [P.R(("PP", b, t0))], "st_pp")

        if SEC < 4: return
        for j in range(3):
            pst = ps[j % 2]
            c0 = NFM * 128 + (j * 128 if j < 2 else 272)
            c0 = c0 - C0SHIFT
            for kc in range(8):
                P.add("pe", (lambda kc, c0, pst: (lambda e: e.matmul(pst[:, 0:Tt], lhsT=W[:, kc, c0:c0 + 128], rhs=hT[:, kc, 0:Tt], start=(kc == 0), stop=(kc == 7))))(kc, c0, pst), [W, hT], [pst])
            if j < 2:
                P.add("act", (lambda j, pst: (lambda e: e.activation(out=zT[:, j, 0:Tt], in_=pst[:, 0:Tt], func=AF.Sigmoid)))(j, pst), [pst], [zT])
                P.add("dve", (lambda j, pst: (lambda e: e.tensor_tensor(out=zT[:, j, 0:Tt], in0=pst[:, 0:Tt], in1=zT[:, j, 0:Tt], op=ALU.mult)))(j, pst), [pst, zT], [zT])
            else:
                P.add("act", (lambda j, pst: (lambda e: e.activation(out=zT[:, j, 0:Tt], in_=pst[:, 0:Tt], func=AF.Copy)))(j, pst), [pst], [zT])
        if SUB < 1: return
        for n in range(nsub):
            pst = ps[2 + (n % 2)]
            pv = ps[5 + (n % 2)]
            for j in range(2):
                P.add("pe", (lambda n, j, pst: (lambda e: e.transpose(out=pst[:, j * 128:(j + 1) * 128], in_=zT[:, j, n * 128:(n + 1) * 128], identity=c.ident[:, :])))(n, j, pst), [zT, c.ident], [pst])
            P.add("pe", (lambda n, pv: (lambda e: e.transpose(out=pv[:, 0:128], in_=zT[:, 2, n * 128:(n + 1) * 128], identity=c.ident[:, :])))(n, pv), [zT, c.ident], [pv])
            P.add("dve", (lambda n, pst: (lambda e: e.tensor_copy(out=st_z[:, n, :], in_=pst[:, 0:256])))(n, pst), [pst], [st_z])
            P.add("act", (lambda n, pv: (lambda e: e.activation(out=st_v[:, n, :], in_=pv[:, 0:128], func=AF.Copy)))(n, pv), [pv], [st_v])
        if SUB < 3: return
        P.dma("sp", m.Zs[b, t0:t0 + Tt, :].rearrange("(n p) f -> p n f", p=128), st_z[:, 0:nsub, :], [st_z], [P.R(("Zs", b, t0))], "st_z")
        P.dma("sp", m.VA[b, t0:t0 + Tt, :].rearrange("(n p) f -> p n f", p=128), st_v[:, 0:nsub, :], [st_v], [P.R(("VA", b, t0))], "st_v")
        if SEC < 5: return
        for j in range(2):
            c0 = NFM * 128 + 256 + j * 8
            for kc in range(8):
                P.add("pe", (lambda kc, c0, j: (lambda e: e.matmul(ps[2 + j][0:8, 0:Tt], lhsT=W[:, kc, c0:c0 + 8], rhs=hT[:, kc, 0:Tt], start=(kc == 0), stop=(kc == 7))))(kc, c0, j), [W, hT], [ps[2 + j]])
        g0, g1, g2 = gt
        P.add("act", lambda e: e.activation(out=g0[0:8, 0:Tt], in_=ps[2][0:8, 0:Tt], func=AF.Identity, bias=m.gcol[0:8, l, 0:1], scale=1.0), [ps[2], m.gcol], [g0])
        P.add("act", lambda e: e.activation(out=g1[0:8, 0:Tt], in_=g0[0:8, 0:Tt], func=AF.Abs), [g0], [g1])
        P.add("act", lambda e: e.activation(out=g1[0:8, 0:Tt], in_=g1[0:8, 0:Tt], func=AF.Exp, scale=-1.0), [g1], [g1])
        P.add("act", lambda e: e.activation(out=g1[0:8, 0:Tt], in_=g1[0:8, 0:Tt], func=AF.Ln, bias=1.0), [g1], [g1])
        P.add("dve", lambda e: e.scalar_tensor_tensor(out=g2[0:8, 0:Tt], in0=g0[0:8, 0:Tt], scalar=0.0, in1=g1[0:8, 0:Tt], op0=ALU.max, op1=ALU.add), [g0, g1], [g2])
        P.add("dve", lambda e: e.tensor_scalar(out=st_g[0:8, 0, 0:Tt], in0=g2[0:8, 0:Tt], scalar1=m.gcol[0:8, l, 1:2], scalar2=None, op0=ALU.mult), [g2, m.gcol], [st_g])
        P.add("act", lambda e: e.activation(out=st_g[0:8, 1, 0:Tt], in_=ps[3][0:8, 0:Tt], func=AF.Sigmoid), [ps[3]], [st_g])
        P.dma("sp", m.G[b].rearrange("(j p) t -> p j t", p=8)[:, :, t0:t0 + Tt], st_g[0:8, :, 0:Tt], [st_g], [P.R(("G", b, t0))], "st_g")

    tl = tl[:NTILES]
    load(0)
    for i, (b, t0, Tt) in enumerate(tl):
        if i + 1 < len(tl):
            load(i + 1)
        body(i, b, t0, Tt)


def stage_attn(m, l, last):
    P, c = m.P, m.c
    NKC = T // 128
    kT = P.sb("kT", [128, 2, T], BF16)
    vS = P.sb("vS", [128, NKC, 2, 65], BF16)
    qTs = [[P.sb(f"qT{i}_{h}", [128, 512], BF16) for h in range(2)] for i in range(2)]
    for i_ in range(2):
        for h_ in range(2):
            P.add("pool", (lambda i_, h_: (lambda e: e.memset(qTs[i_][h_][:, :], 0.0)))(i_, h_), [], [qTs[i_][h_]])
    pTs = [P.sb(f"pT{i}", [128, 1024], BF16) for i in range(3)]
    rdn = P.sb("rdn", [128, 512], F32)
    oS = P.sb("oS", [64, 512], F32)
    ys = [P.sb(f"ys{i}", [64, 512], BF16) for i in range(2)]
    ps_s = [P.ps(f"ps_s{i}", [128, 1024]) for i in range(2)]
    ps_o = [P.ps(f"ps_o{i}", [128, 512]) for i in range(2)]
    ps_b = P.ps("ps_b", [128, 512])
    cnt = [0, 0]
    for b in range(2):
        P.add("pool", lambda e: e.memset(vS[:, :, :, 64:65], 1.0), [], [vS])
        for s_ in range(2):
            P.dma("sp", kT[:, s_, :], m.KA[b, s_], [P.R(("KA", b, t0)) for (t0, _) in tiles_of_batch()], [kT], "kT")
        for h_ in range(2):
            P.dma("sp", vS[:, :, h_, 0:64], m.VA[b][:, h_ * 64:(h_ + 1) * 64].rearrange("(n p) d -> p n d", p=128), [P.R(("VA", b, t0)) for (t0, _) in tiles_of_batch()], [vS], "vS")
        work = [(qc, t0, Tt) for qc in range(4) for (t0, Tt) in tiles_of_batch() if not (last and t0 < CT)]

        def loadq(i):
            qc, t0, Tt = work[i]
            for h_ in range(2):
                P.dma("sp", qTs[i % 2][h_][h_ * 64:(h_ + 1) * 64, 0:Tt], m.QA[b][qc * 128 + h_ * 64:qc * 128 + (h_ + 1) * 64, t0:t0 + Tt], [P.R(("QA", b, t0))], [qTs[i % 2][h_]], ("ldq", i % 2, h_))

        def body(i, qc, t0, Tt):
            kv = qc // 2
            kcs = list(range(2)) if t0 < CT else list(range(NKC))
            def head(hh):
                idx = 0 if kv == hh else 1
                qT = qTs[i % 2][hh]
                po = ps_o[hh]
                r0 = hh * 64
                ND_ = 2
                nd = len(kcs) // 2
                base_ = cnt[0]
                cnt[0] += nd

                def s_stage(n_):
                    pss = ps_s[(base_ + n_) % ND_]
                    pT = pTs[(base_ + n_) % 3]
                    for u_ in range(2):
                        kc = kcs[2 * n_ + u_]
                        P.add("pe", (lambda kc, u_: (lambda e: e.matmul(pss[:, u_ * 512:u_ * 512 + Tt], lhsT=kT[:, idx, kc * 128:(kc + 1) * 128], rhs=qT[:, 0:Tt], start=True, stop=True)))(kc, u_), [kT, qT], [pss])
                    if Tt == 512:
                        P.add("act", lambda e: e.activation(out=pT[:, :], in_=pss[:, :], func=AF.Exp, scale=0.125), [pss], [pT])
                    else:
                        P.add("act", lambda e: e.activation(out=pT[:, :].rearrange("p (u t) -> p u t", u=2)[:, :, 0:Tt], in_=pss[:, :].rearrange("p (u t) -> p u t", u=2)[:, :, 0:Tt], func=AF.Exp, scale=0.125), [pss], [pT])

                def pv_stage(n_):
                    pT = pTs[(base_ + n_) % 3]
                    for u_ in range(2):
                        kc = kcs[2 * n_ + u_]
                        P.add("pe", (lambda kc, u_: (lambda e: e.matmul(po[0:65, 0:Tt], lhsT=vS[:, kc, kv, 0:65], rhs=pT[:, u_ * 512:u_ * 512 + Tt], start=(n_ == 0 and u_ == 0), stop=(n_ == nd - 1 and u_ == 1))))(kc, u_), [vS, pT], [po])

                s_stage(0)
                for n_ in range(nd):
                    if n_ + 1 < nd:
                        s_stage(n_ + 1)
                    pv_stage(n_)
                y = ys[cnt[1] % 2]
                cnt[1] += 1
                P.add("dve", lambda e: e.reciprocal(out=rdn[64:65, 0:Tt], in_=po[64:65, 0:Tt]), [po], [rdn])
                P.add("pe", lambda e: e.matmul(ps_b[0:64, 0:Tt], lhsT=c.onesf[64:65, 0:64], rhs=rdn[64:65, 0:Tt], start=True, stop=True), [rdn, c.onesf], [ps_b])
                P.add("act", lambda e: e.activation(out=oS[:, 0:Tt], in_=po[0:64, 0:Tt], func=AF.Copy), [po], [oS])
                P.add("dve", (lambda y: (lambda e: e.tensor_tensor(out=y[:, 0:Tt], in0=ps_b[0:64, 0:Tt], in1=oS[:, 0:Tt], op=ALU.mult)))(y), [ps_b, oS], [y])
                hrow = 256 + (2 * qc + hh) * 64
                P.dma("sp", m.YT[b][hrow:hrow + 64, t0:t0 + Tt], y[:, 0:Tt], [y], [P.R(("YT", b, t0, hrow))], ("sty", cnt[1] % 2))

            head(0)
            head(1)

        loadq(0)
        for i, (qc, t0, Tt) in enumerate(work):
            if i + 1 < len(work):
                loadq(i + 1)
            body(i, qc, t0, Tt)


def epilogue(m):
    P, c = m.P, m.c
    alloc_norm(m)
    xts = [P.sb(f"ext{i}", [128, 8, 512], F32) for i in range(2)]
    sq = P.sb("esq", [128, 8, 512], BF16)
    yT = P.sb("eyT", [128, 8, 512], F32)
    ost = [P.sb(f"eost{i}", [128, D], F32) for i in range(2)]
    ps = [P.ps(f"eps{i}", [128, 512]) for i in range(8)]
    tl = [(b, t0, Tt) for b in range(2) for (t0, Tt) in tiles_of_batch() if t0 >= CT]
    cnt = [0]

    def load(i):
        b, t0, Tt = tl[i]
        P.dma("sp", xts[i % 2][:, :, 0:Tt], m.XT[b].rearrange("(c p) t -> p c t", p=128)[:, :, t0:t0 + Tt], [P.R(("XT", b, t0))], [xts[i % 2]], ("ldx", i % 2))

    def body(i, b, t0, Tt):
        xt = xts[i % 2]
        pss = ps[7]
        P.add("act", lambda e: e.activation(out=sq[:, :, 0:Tt], in_=xt[:, :, 0:Tt], func=AF.Square), [xt], [sq])
        for kc in range(8):
            P.add("pe", (lambda kc: (lambda e: e.matmul(pss[:, 0:Tt], lhsT=c.onesb[:, :], rhs=sq[:, kc, 0:Tt], start=(kc == 0), stop=(kc == 7))))(kc), [sq, c.onesb], [pss])
        rs = m.rs
        P.add("act", lambda e: e.activation(out=rs[:, 0:Tt], in_=pss[:, 0:Tt], func=AF.Sqrt, scale=1.0 / D, bias=m.epsc[:, 0:1]), [pss, m.epsc], [rs])
        P.add("dve", lambda e: e.reciprocal(out=rs[:, 0:Tt], in_=rs[:, 0:Tt]), [rs], [rs])
        for kc in range(8):
            P.add("dve", (lambda kc: (lambda e: e.scalar_tensor_tensor(out=yT[:, kc, 0:Tt], in0=xt[:, kc, 0:Tt], scalar=c.vcol[:, 0, R_NF + kc:R_NF + kc + 1], in1=rs[:, 0:Tt],
                                                                       op0=ALU.mult, op1=ALU.mult)))(kc), [xt, rs, c.vcol], [yT])
        for n in range(Tt // 128):
            o = ost[cnt[0] % 2]
            cnt[0] += 1
            for half in range(2):
                pt = ps[2 * (n % 2) + half]
                for j in range(4):
                    kc = half * 4 + j
                    P.add("pe", (lambda kc, j, pt, n: (lambda e: e.transpose(out=pt[:, j * 128:(j + 1) * 128], in_=yT[:, kc, n * 128:(n + 1) * 128], identity=c.ident[:, :])))(kc, j, pt, n), [yT, c.ident], [pt])
                if half == 0:
                    P.add("dve", (lambda pt, o: (lambda e: e.tensor_copy(out=o[:, 0:512], in_=pt[:, 0:512])))(pt, o), [pt], [o])
                else:
                    P.add("act", (lambda pt, o: (lambda e: e.activation(out=o[:, 512:1024], in_=pt[:, 0:512], func=AF.Copy)))(pt, o), [pt], [o])
            tt = t0 - CT + n * 128
            P.dma("sp", m.out[b, tt:tt + 128, :], o[:, :], [o], [P.R(("out", b, tt))], ("sto", cnt[0] % 2))

    load(0)
    for i, (b, t0, Tt) in enumerate(tl):
        if i + 1 < len(tl):
            load(i + 1)
        body(i, b, t0, Tt)


def ffn_blocks(m, l):
    i = l // 2
    blocks = []
    if l % 2 == 0:
        for (f0, nf) in ((0, 6), (768, 5), (1408, 6), (2176, 5)):
            w = nf * 128
            blocks.append((m.ffn_g_b[i][:, f0:f0 + w], m.ffn_u_b[i][:, f0:f0 + w], m.ffn_d_b[i][f0:f0 + w, :], nf, None,
                           [("ffn_g_b", i), ("ffn_u_b", i), ("ffn_d_b", i)]))
    else:
        for e in range(NE):
            for (f0, nf) in ((0, 6), (768, 5)):
                w = nf * 128
                blocks.append((m.moe_g_b[i, e][:, f0:f0 + w], m.moe_u_b[i, e][:, f0:f0 + w], m.moe_d_b[i, e][f0:f0 + w, :], nf, e,
                               [("moe_g_b", i, e), ("moe_u_b", i, e), ("moe_d_b", i, e)]))
    return blocks


def stage_ffn(m, l, last):
    P, c = m.P, m.c
    moe = (l % 2 == 1)
    alloc_norm(m)
    Wout = P.sb("Wout", [128, 8, D], BF16)
    P.dma("sp", Wout[:, :, :], m.w_out_b[l].rearrange("(k p) n -> p k n", p=128), [P.R(("w_out_b", l))], [Wout], "Wout")
    wg = [P.sb(f"wg{i}", [128, 8, 768], BF16) for i in range(2)]
    wu = [P.sb(f"wu{i}", [128, 8, 768], BF16) for i in range(2)]
    wd = [P.sb(f"wd{i}", [128, 6, D], BF16) for i in range(2)]
    xt = P.sb("fxt", [128, 8, 512], F32)
    yT = P.sb("fyT", [128, 8, 512], BF16)
    cb = P.sb("fcb", [128, 2, 512], F32)
    pp = P.sb("fpp", [128, 2, 514], F32)
    cacc = P.sb("fcacc", [128, 512], F32)
    sq = P.sb("fsq", [128, 8, 512], BF16)
    acc = P.sb("facc", [128, 8, 512], F32)
    h2b = P.sb("fh2b", [128, 8, 512], BF16)
    act = P.sb("fact", [128, 6, 512], BF16)
    sg = [P.sb(f"fsg{i}", [128, 512], F32) for i in range(2)]
    a1 = P.sb("fa1", [128, 512], F32)
    cB = P.sb("fcB", [128, 512], F32)
    combT = P.sb("fcombT", [8, 512], F32)
    combs = P.sb("fcombs", [128, 4, 8], F32)
    rt = [P.sb(f"frt{i}", [128, 8], F32) for i in range(4)]
    rsc = [P.sb(f"frsc{i}", [128, 1], F32) for i in range(3)]
    if moe:
        Rw = P.sb("fRw", [128, 8, NE], F32)
        P.dma("sp", Rw[:, :, :], m.router[l // 2].rearrange("(k p) n -> p k n", p=128), [], [Rw], "Rw")
    pg = [P.ps(f"fpg{i}", [128, 512]) for i in range(2)]
    pu = [P.ps(f"fpu{i}", [128, 512]) for i in range(2)]
    pd = [P.ps(f"fpd{i}", [128, 512]) for i in range(2)]
    pm = [P.ps(f"fpm{i}", [128, 512]) for i in range(2)]
    blocks = ffn_blocks(m, l)
    groups = [(b, t0, Tt) for b in range(2) for (t0, Tt) in tiles_of_batch() if not (last and t0 < CT)]
    seq = [(gi, bi) for gi in range(len(groups)) for bi in range(len(blocks))]
    cnt = [0]

    def loadw(si):
        gi, bi = seq[si]
        gs, us, ds, nf, e, keys = blocks[bi]
        k = si % 2
        rd = [P.R(kk) for kk in keys]
        P.dma("sp", wg[k][:, :, 0:nf * 128], gs.rearrange("(k p) n -> p k n", p=128), rd, [wg[k]], ("wg", k))
        P.dma("sp", wu[k][:, :, 0:nf * 128], us.rearrange("(k p) n -> p k n", p=128), rd, [wu[k]], ("wu", k))
        P.dma("sp", wd[k][:, 0:nf, :], ds.rearrange("(f p) n -> p f n", p=128), rd, [wd[k]], ("wd", k))

    def group(gi, b, t0, Tt):
        which = 2 if t0 < CT else b
        XTv = m.XT[b].rearrange("(c p) t -> p c t", p=128)
        P.dma("sp", xt[:, :, 0:Tt], XTv[:, :, t0:t0 + Tt], [P.R(("XT", b, t0))], [xt], "fxt")
        P.dma("sp", yT[:, 0:6, 0:Tt], m.YT[b][0:768, :].rearrange("(c p) t -> p c t", p=128)[:, :, t0:t0 + Tt], [P.R(("YT", b, t0))], [yT], "fyT")
        P.dma("sp", cb[:, :, 0:Tt], m.CB[b].rearrange("(c p) t -> p c t", p=128)[:, :, t0:t0 + Tt], [P.R(("CB", b, t0))], [cb], "fcb")
        P.dma("sp", pp[:, :, 0:Tt + 2], m.PP[b].rearrange("(c p) t -> p c t", p=128)[:, :, pc(t0) - 1:pc(t0) + Tt + 1], [P.R(("PP", b, t0))], [pp], "fpp")
        for j in range(2):
            w = lambda k, j=j: c.vcol[:, l, R_CC + k * 2 + j:R_CC + k * 2 + j + 1]
            P.add("dve", (lambda j: (lambda e: e.tensor_scalar(out=cacc[:, 0:Tt], in0=pp[:, j, 0:Tt], scalar1=w(0, j), scalar2=None, op0=ALU.mult)))(j), [pp, c.vcol], [cacc])
            P.add("dve", (lambda j: (lambda e: e.scalar_tensor_tensor(out=cacc[:, 0:Tt], in0=pp[:, j, 1:Tt + 1], scalar=w(1, j), in1=cacc[:, 0:Tt], op0=ALU.mult, op1=ALU.add)))(j), [pp, cacc, c.vcol], [cacc])
            P.add("dve", (lambda j: (lambda e: e.scalar_tensor_tensor(out=cacc[:, 0:Tt], in0=pp[:, j, 2:Tt + 2], scalar=w(2, j), in1=cacc[:, 0:Tt], op0=ALU.mult, op1=ALU.add)))(j), [pp, cacc, c.vcol], [cacc])
            P.add("dve", (lambda j: (lambda e: e.tensor_tensor(out=yT[:, 6 + j, 0:Tt], in0=cacc[:, 0:Tt], in1=cb[:, j, 0:Tt], op=ALU.mult)))(j), [cacc, cb], [yT])
        for oc in range(8):
            ps_ = pm[oc % 2]
            for kc in range(8):
                P.add("pe", (lambda oc, kc, ps_: (lambda e: e.matmul(ps_[:, 0:Tt], lhsT=Wout[:, kc, oc * 128:(oc + 1) * 128], rhs=yT[:, kc, 0:Tt], start=(kc == 0), stop=(kc == 7))))(oc, kc, ps_), [Wout, yT], [ps_])
            P.add("dve", (lambda oc, ps_: (lambda e: e.scalar_tensor_tensor(out=xt[:, oc, 0:Tt], in0=ps_[:, 0:Tt], scalar=c.modv[:, l, 16 + oc, which:which + 1], in1=xt[:, oc, 0:Tt],
                                                                            op0=ALU.mult, op1=ALU.add)))(oc, ps_), [ps_, xt, c.modv], [xt])
        norm_mod(m, l, which, xt, sq, h2b, pm[0], Tt, c.gm2, 24, hf=(acc if moe else None))
        if moe:
            nsub = Tt // 128

            def route(n):
                pr = pm[1]
                for kc in range(8):
                    P.add("pe", (lambda kc: (lambda e: e.matmul(pr[:, 0:NE], lhsT=acc[:, kc, n * 128:(n + 1) * 128], rhs=Rw[:, kc, :], start=(kc == 0), stop=(kc == 7))))(kc), [acc, Rw], [pr])
                mx, ex, mk, em = rt
                nm1, ss, ri = rsc
                P.add("dve", lambda e: e.max(out=mx[:, :], in_=pr[:, 0:NE]), [pr], [mx])
                P.add("dve", lambda e: e.tensor_scalar(out=nm1[:, :], in0=mx[:, 0:1], scalar1=-1.0, scalar2=None, op0=ALU.mult), [mx], [nm1])
                P.add("act", lambda e: e.activation(out=ex[:, :], in_=pr[:, 0:NE], func=AF.Exp, bias=nm1[:, 0:1], scale=1.0), [pr, nm1], [ex])
                P.add("dve", lambda e: e.tensor_scalar(out=mk[:, :], in0=pr[:, 0:NE], scalar1=mx[:, 1:2], scalar2=None, op0=ALU.is_ge), [pr, mx], [mk])
                P.add("dve", lambda e: e.tensor_tensor(out=em[:, :], in0=ex[:, :], in1=mk[:, :], op=ALU.mult), [ex, mk], [em])
                P.add("dve", lambda e: e.reduce_sum(out=ss[:, :], in_=em[:, :], axis=AX.X), [em], [ss])
                P.add("dve", lambda e: e.reciprocal(out=ri[:, :], in_=ss[:, :]), [ss], [ri])
                P.add("dve", lambda e: e.tensor_scalar(out=combs[:, n, :], in0=em[:, :], scalar1=ri[:, 0:1], scalar2=None, op0=ALU.mult), [em, ri], [combs])

            for n in range(nsub):
                route(n)
            pt = pm[1]
            for n in range(nsub):
                P.add("pe", (lambda n: (lambda e: e.transpose(out=pt[0:NE, n * 128:(n + 1) * 128], in_=combs[:, n, :], identity=c.ident[:, :])))(n), [combs, c.ident], [pt])
            P.add("dve", lambda e: e.tensor_copy(out=combT[:, 0:Tt], in_=pt[0:NE, 0:Tt]), [pt], [combT])

        def block(bi):
            si = cnt[0]
            cnt[0] += 1
            if si + 1 < len(seq):
                loadw(si + 1)
            gs, us, ds, nf, ex_, keys = blocks[bi]
            k = si % 2
            G_, U_, D_ = wg[k], wu[k], wd[k]
            if ex_ is not None and (bi % 2 == 0):
                pcb = pm[1]
                P.add("pe", lambda e: e.matmul(pcb[:, 0:Tt], lhsT=c.sel[0:NE, ex_, :], rhs=combT[0:NE, 0:Tt], start=True, stop=True), [combT, c.sel], [pcb])
                P.add("act", lambda e: e.activation(out=cB[:, 0:Tt], in_=pcb[:, 0:Tt], func=AF.Copy), [pcb], [cB])

            def fchunk(fc):
                g_, u_, s_ = pg[fc % 2], pu[fc % 2], sg[fc % 2]
                for kc in range(8):
                    P.add("pe", (lambda kc: (lambda e: e.matmul(g_[:, 0:Tt], lhsT=G_[:, kc, fc * 128:(fc + 1) * 128], rhs=h2b[:, kc, 0:Tt], start=(kc == 0), stop=(kc == 7))))(kc), [G_, h2b], [g_])
                for kc in range(8):
                    P.add("pe", (lambda kc: (lambda e: e.matmul(u_[:, 0:Tt], lhsT=U_[:, kc, fc * 128:(fc + 1) * 128], rhs=h2b[:, kc, 0:Tt], start=(kc == 0), stop=(kc == 7))))(kc), [U_, h2b], [u_])
                P.add("act", lambda e: e.activation(out=s_[:, 0:Tt], in_=g_[:, 0:Tt], func=AF.Silu), [g_], [s_])
                if ex_ is None:
                    P.add("dve", lambda e: e.tensor_tensor(out=act[:, fc, 0:Tt], in0=u_[:, 0:Tt], in1=s_[:, 0:Tt], op=ALU.mult), [u_, s_], [act])
                else:
                    P.add("dve", lambda e: e.tensor_tensor(out=a1[:, 0:Tt], in0=u_[:, 0:Tt], in1=s_[:, 0:Tt], op=ALU.mult), [u_, s_], [a1])
                    P.add("pool", lambda e: e.tensor_tensor(out=act[:, fc, 0:Tt], in0=a1[:, 0:Tt], in1=cB[:, 0:Tt], op=ALU.mult), [a1, cB], [act])

            for fc in range(nf):
                fchunk(fc)

            def dchunk(oc):
                d_ = pd[oc % 2]
                for fc in range(nf):
                    P.add("pe", (lambda fc: (lambda e: e.matmul(d_[:, 0:Tt], lhsT=D_[:, fc, oc * 128:(oc + 1) * 128], rhs=act[:, fc, 0:Tt], start=(fc == 0), stop=(fc == nf - 1))))(fc), [D_, act], [d_])
                if bi == 0:
                    P.add("act", lambda e: e.activation(out=acc[:, oc, 0:Tt], in_=d_[:, 0:Tt], func=AF.Copy), [d_], [acc])
                else:
                    P.add("dve", lambda e: e.tensor_tensor(out=acc[:, oc, 0:Tt], in0=d_[:, 0:Tt], in1=acc[:, oc, 0:Tt], op=ALU.add), [d_, acc], [acc])

            for oc in range(8):
                dchunk(oc)

        for bi in range(len(blocks)):
            block(bi)
        for oc in range(8):
            P.add("dve", (lambda oc: (lambda e: e.scalar_tensor_tensor(out=xt[:, oc, 0:Tt], in0=acc[:, oc, 0:Tt], scalar=c.modv[:, l, 40 + oc, which:which + 1], in1=xt[:, oc, 0:Tt],
                                                                       op0=ALU.mult, op1=ALU.add)))(oc), [acc, xt, c.modv], [xt])
        P.dma("sp", XTv[:, :, t0:t0 + Tt], xt[:, :, 0:Tt], [xt], [P.R(("XT", b, t0))], "fxs")

    loadw(0)
    for gi, (b, t0, Tt) in enumerate(groups):
        group(gi, b, t0, Tt)


def stage_gdn_pre(m, l):
    P, c = m.P, m.c
    qin = [P.sb(f"gqin{i}", [128, 6, 514], F32) for i in range(2)]
    s = P.sb("gs", [128, 6, 512], F32)
    cacc = [P.sb(f"gcacc{i}", [128, 512], F32) for i in range(2)]
    sqq = P.sb("gsqq", [128, 512], BF16)
    rn = P.sb("grn", [128, 512], F32)
    kf = P.sb("gkf", [128, 2, 512], F32)
    qb = P.sb("gqb", [128, 2, 512], BF16)
    kb = P.sb("gkb", [128, 2, 512], BF16)
    tk = [P.sb(f"gtk{i}", [128, 512], F32) for i in range(2)]
    epsc = P.sb("gepsc", [128, 1], F32)
    P.add("pool", lambda e: e.memset(epsc[:, :], EPS), [], [epsc])
    ps = [P.ps(f"gps{i}", [128, 512]) for i in range(4)]
    tl = [(b, t0, Tt) for b in range(2) for (t0, Tt) in tiles_of_batch()]
    cnt = [0]

    def load(i):
        b, t0, Tt = tl[i]
        P.dma("sp", qin[i % 2][:, :, 0:Tt + 2], m.QKVp[b].rearrange("(c p) t -> p c t", p=128)[:, :, pc(t0) - 1:pc(t0) + Tt + 1], [P.R(("QKVp", b, t0))], [qin[i % 2]], ("gld", i % 2))

    def body(i, b, t0, Tt):
        x = qin[i % 2]

        def conv(j):
            a = cacc[j % 2]
            w = lambda k: c.vcol[:, l, R_CA + k * 6 + j:R_CA + k * 6 + j + 1]
            P.add("dve", lambda e: e.tensor_scalar(out=a[:, 0:Tt], in0=x[:, j, 0:Tt], scalar1=w(0), scalar2=None, op0=ALU.mult), [x, c.vcol], [a])
            P.add("dve", lambda e: e.scalar_tensor_tensor(out=a[:, 0:Tt], in0=x[:, j, 1:Tt + 1], scalar=w(1), in1=a[:, 0:Tt], op0=ALU.mult, op1=ALU.add), [x, a, c.vcol], [a])
            P.add("dve", lambda e: e.scalar_tensor_tensor(out=a[:, 0:Tt], in0=x[:, j, 2:Tt + 2], scalar=w(2), in1=a[:, 0:Tt], op0=ALU.mult, op1=ALU.add), [x, a, c.vcol], [a])
            P.add("act", lambda e: e.activation(out=s[:, j, 0:Tt], in_=a[:, 0:Tt], func=AF.Silu), [a], [s])

        for j in range(6):
            conv(j)

        def l2n(j):
            pss = ps[j % 2]
            P.add("act", lambda e: e.activation(out=sqq[:, 0:Tt], in_=s[:, j, 0:Tt], func=AF.Square), [s], [sqq])
            P.add("pe", lambda e: e.matmul(pss[:, 0:Tt], lhsT=c.bdiag[:, :], rhs=sqq[:, 0:Tt], start=True, stop=True), [sqq, c.bdiag], [pss])
            P.add("act", lambda e: e.activation(out=rn[:, 0:Tt], in_=pss[:, 0:Tt], func=AF.Sqrt, scale=1.0, bias=epsc[:, 0:1]), [pss, epsc], [rn])
            P.add("dve", lambda e: e.reciprocal(out=rn[:, 0:Tt], in_=rn[:, 0:Tt]), [rn], [rn])
            if j < 2:
                P.add("dve", lambda e: e.scalar_tensor_tensor(out=qb[:, j, 0:Tt], in0=s[:, j, 0:Tt], scalar=0.125, in1=rn[:, 0:Tt], op0=ALU.mult, op1=ALU.mult), [s, rn], [qb])
            else:
                P.add("dve", lambda e: e.tensor_tensor(out=kf[:, j - 2, 0:Tt], in0=s[:, j, 0:Tt], in1=rn[:, 0:Tt], op=ALU.mult), [s, rn], [kf])
                P.add("pool", lambda e: e.tensor_copy(out=kb[:, j - 2, 0:Tt], in_=kf[:, j - 2, 0:Tt]), [kf], [kb])

        for j in range(4):
            l2n(j)
        P.dma("sp", m.QG[b].rearrange("(c p) t -> p c t", p=128)[:, :, t0:t0 + Tt], qb[:, :, 0:Tt], [qb], [P.R(("QG", b, t0))], "gqb")
        P.dma("sp", m.KG[b].rearrange("(c p) t -> p c t", p=128)[:, :, t0:t0 + Tt], kb[:, :, 0:Tt], [kb], [P.R(("KG", b, t0))], "gkb")

        def tok(n):
            pt = ps[2 + n % 2]
            o = tk[cnt[0] % 2]
            cnt[0] += 1
            for j in range(2):
                P.add("pe", (lambda j: (lambda e: e.transpose(out=pt[:, j * 128:(j + 1) * 128], in_=kf[:, j, n * 128:(n + 1) * 128], identity=c.ident[:, :])))(j), [kf, c.ident], [pt])
                P.add("pe", (lambda j: (lambda e: e.transpose(out=pt[:, 256 + j * 128:256 + (j + 1) * 128], in_=s[:, 4 + j, n * 128:(n + 1) * 128], identity=c.ident[:, :])))(j), [s, c.ident], [pt])
            P.add("act", lambda e: e.activation(out=o[:, :], in_=pt[:, :], func=AF.Copy), [pt], [o])
            tt = t0 + n * 128
            P.dma("sp", m.KGt[b, tt:tt + 128, :], o[:, 0:256], [o], [P.R(("KGt", b, tt))], ("gtk", cnt[0] % 2))
            P.dma("sp", m.VGt[b, tt:tt + 128, :], o[:, 256:512], [o], [P.R(("VGt", b, tt))], ("gtk", cnt[0] % 2))

        for n in range(Tt // 128):
            tok(n)

    load(0)
    for i, (b, t0, Tt) in enumerate(tl):
        if i + 1 < len(tl):
            load(i + 1)
        body(i, b, t0, Tt)


def bc3(ap, n, w):
    return ap.unsqueeze(2).to_broadcast([ap.shape[0], n, w])


def bcm(ap, n):
    return ap.unsqueeze(1).to_broadcast([ap.shape[0], n, ap.shape[1]])


def stage_gdn_scan(m, l):
    P, c = m.P, m.c
    DCONST = {0: ((c.Ls, c.Ui, c.Ui, c.Us, c.Ui), list(range(0, T // CHK))),
              1: ((c.Us, c.Li, c.Li, c.Ls, c.Li), [1, 0] + list(range(T // CHK - 1, 1, -1)))}
    H = 4
    psr = [P.ps(f"spsr{i}", [128, 512]) for i in range(7)]
    ptr_sh = P.ps("sptr_sh", [128, 1024], BF16)
    pctr = [0]
    th = []
    gm4 = P.sb("gm4", [128, 7, H * CHK], BF16)
    for k_ in range(7):
        P.add("pool", (lambda k_: (lambda e: e.tensor_copy(out=gm4[:, k_, :].rearrange("p (h c) -> p h c", h=H), in_=bcm(c.gmask[:, k_, :], H))))(k_), [c.gmask], [gm4])
    P.add("pool", lambda e: e.tensor_scalar(out=gm4[:, 1:7, :], in0=gm4[:, 1:7, :], scalar1=-1.0, scalar2=None, op0=ALU.mult), [gm4], [gm4])
    for (b, d_) in ((0, 0), (1, 0), (0, 1), (1, 1)):
        t = M()
        t.b = b
        t.d = d_
        sb = lambda n, sh, dt, b=b, d_=d_: P.sb(f"s{b}{d_}{n}", sh, dt)
        t.kT = [sb(f"kT{i}", [64, H, CHK], BF16) for i in range(2)]
        t.qT = [sb(f"qT{i}", [64, H, CHK], BF16) for i in range(2)]
        t.kt = [sb(f"kt{i}", [128, H, 64], F32) for i in range(2)]
        t.vt = [sb(f"vt{i}", [128, H, 64], F32) for i in range(2)]
        t.gT = [sb(f"gT{i}", [16, CHK], F32) for i in range(2)]
        t.g = sb("g", [128, 16], F32)
        t.gc8 = sb("gc8", [128, 8], F32)
        t.ge = sb("ge", [128, 12], F32)
        t.E12 = sb("E12", [128, 12], F32)
        t.rarg = sb("rarg", [128, H * CHK], F32)
        t.Ee = sb("Ee", [128, H, CHK], F32)
        t.Di = sb("Di", [128, H, CHK], F32)
        t.Ds = sb("Ds", [128, H, CHK], F32)
        t.tmp = sb("tmp", [128, H, CHK], F32)
        t.X = [sb(f"X{i}", [128, H, CHK], BF16) for i in range(2)]
        t.XT = [sb(f"XT{i}", [128, H, CHK], BF16) for i in range(2)]
        t.PT = [sb(f"PT{i}", [128, H, CHK], BF16) for i in range(2)]
        t.IX = sb("IX", [128, H, CHK], BF16)
        t.U = sb("U", [128, H, CHK], BF16)
        t.UT = sb("UT", [128, H, CHK], BF16)
        t.QK = sb("QK", [128, H, CHK], BF16)
        t.t2 = sb("t2", [128, H, 64], F32)
        t.rb = sb("rb", [128, H, 64], BF16)
        t.vn = sb("vn", [128, H, 64], BF16)
        t.kd = sb("kd", [128, H, 64], BF16)
        t.o = [sb(f"o{i}", [128, H, 64], F32) for i in range(2)]
        t.S = sb("S", [64, H, 64], F32)
        t.Sb = sb("Sb", [64, H, 64], BF16)
        t.t4 = sb("t4", [64, H, 64], F32)
        t.ptr = ptr_sh
        P.add("pool", (lambda t: (lambda e: e.memset(t.S[:, :, :], 0.0)))(t), [], [t.S])
        P.add("pool", (lambda t: (lambda e: e.memset(t.Sb[:, :, :], 0.0)))(t), [], [t.Sb])
        th.append(t)

    def bank(t):
        p = psr[pctr[0] % 7]
        pctr[0] += 1
        return p

    def load(t, si):
        cc = DCONST[t.d][1][si]
        t0 = cc * CHK
        b, k = t.b, si % 2
        tl0 = 0 if t0 < CT else CT + ((t0 - CT) // 512) * 512
        P.dma("sp", t.kT[k][:, :, :], m.KG[b].rearrange("(h d) t -> d h t", d=64)[:, :, t0:t0 + CHK], [P.R(("KG", b, tl0))], [t.kT[k]], ("skT", b, t.d, k))
        P.dma("sp", t.qT[k][:, :, :], m.QG[b].rearrange("(h d) t -> d h t", d=64)[:, :, t0:t0 + CHK], [P.R(("QG", b, tl0))], [t.qT[k]], ("sqT", b, t.d, k))
        P.dma("sp", t.kt[k][:, :, :], m.KGt[b, t0:t0 + CHK, :].rearrange("p (h d) -> p h d", d=64), [P.R(("KGt", b, t0))], [t.kt[k]], ("skt", b, t.d, k))
        P.dma("sp", t.vt[k][:, :, :], m.VGt[b, t0:t0 + CHK, :].rearrange("p (h d) -> p h d", d=64), [P.R(("VGt", b, t0))], [t.vt[k]], ("svt", b, t.d, k))
        P.dma("sp", t.gT[k][:, :], m.G[b][:, t0:t0 + CHK], [P.R(("G", b, tl0))], [t.gT[k]], ("sgT", b, t.d, k))

    def step(t, si):
        d = t.d
        (LA, M1, INC, STR, LGC), order = DCONST[d]
        cc = order[si]
        t0 = cc * CHK
        k = si % 2
        kT, qT, kt, vt, gT = t.kT[k], t.qT[k], t.kt[k], t.vt[k], t.gT[k]
        la = t.g[:, d * 4:d * 4 + 4]
        be = t.g[:, 8 + d * 4:8 + d * 4 + 4]
        fs = []

        def f_gates():
            p = bank(t)
            P.add("pe", lambda e: e.transpose(out=p[:, 0:16], in_=gT[:, :], identity=c.ident[0:16, 0:16]), [gT, c.ident], [p])
            P.add("dve", lambda e: e.tensor_copy(out=t.g[:, :], in_=p[:, 0:16]), [p], [t.g])
            p2 = bank(t)
            P.add("pe", lambda e: e.matmul(p2[:, 0:4], lhsT=LGC[:, :], rhs=la, start=True, stop=True), [t.g, LGC], [p2])
            P.add("pe", lambda e: e.matmul(p2[:, 4:8], lhsT=c.onesf[:, :], rhs=la, start=True, stop=True), [t.g, c.onesf], [p2])
            P.add("dve", lambda e: e.tensor_copy(out=t.gc8[:, :], in_=p2[:, 0:8]), [p2], [t.gc8])
            P.add("pool", lambda e: e.tensor_copy(out=t.ge[:, 0:4], in_=t.gc8[:, 0:4]), [t.gc8], [t.ge])
            P.add("pool", lambda e: e.tensor_tensor(out=t.ge[:, 4:8], in0=t.gc8[:, 4:8], in1=t.gc8[:, 0:4], op=ALU.subtract), [t.gc8], [t.ge])
            P.add("pool", lambda e: e.tensor_copy(out=t.ge[:, 8:12], in_=t.gc8[:, 4:8]), [t.gc8], [t.ge])
            P.add("act", lambda e: e.activation(out=t.E12[:, :], in_=t.ge[:, :], func=AF.Exp), [t.ge], [t.E12])
            P.add("dve", lambda e: e.tensor_tensor(out=t.rarg[:, :].rearrange("p (h c) -> p h c", h=H), in0=bcm(M1[:, :], H), in1=bc3(la, H, CHK), op=ALU.mult), [M1, t.g], [t.rarg])
        fs.append(f_gates)

        def f_arg():
            p = bank(t)
            P.add("pe", lambda e: e.matmul(p[:, :], lhsT=LA[:, :], rhs=t.rarg[:, :], start=True, stop=True), [t.rarg, LA], [p])
            P.add("act", lambda e: e.activation(out=t.Ee[:, :, :], in_=p[:, :], func=AF.Exp), [p], [t.Ee])
            P.add("pool", lambda e: e.tensor_tensor(out=t.Di[:, :, :], in0=t.Ee[:, :, :], in1=bcm(INC[:, :], H), op=ALU.mult), [t.Ee, INC], [t.Di])
            P.add("pool", lambda e: e.tensor_tensor(out=t.Ds[:, :, :], in0=t.Ee[:, :, :], in1=bcm(STR[:, :], H), op=ALU.mult), [t.Ee, STR], [t.Ds])
        fs.append(f_arg)

        def f_kk():
            pk = bank(t)
            for h in range(H):
                P.add("pe", (lambda h: (lambda e: e.matmul(pk[:, h * CHK:(h + 1) * CHK], lhsT=kT[:, h, :], rhs=kT[:, h, :], start=True, stop=True)))(h), [kT], [pk])
            P.add("dve", lambda e: e.tensor_tensor(out=t.tmp[:, :, :], in0=pk[:, :], in1=t.Ds[:, :, :], op=ALU.mult), [pk, t.Ds], [t.tmp])
            P.add("pool", lambda e: e.tensor_tensor(out=t.XT[0][:, :, :], in0=t.tmp[:, :, :], in1=bc3(be, H, CHK), op=ALU.mult), [t.tmp, t.g], [t.XT[0]])
            pq = bank(t)
            for h in range(H):
                P.add("pe", (lambda h: (lambda e: e.matmul(pq[:, h * CHK:(h + 1) * CHK], lhsT=kT[:, h, :], rhs=qT[:, h, :], start=True, stop=True)))(h), [kT, qT], [pq])
            P.add("dve", lambda e: e.tensor_tensor(out=t.QK[:, :, :], in0=pq[:, :], in1=t.Di[:, :, :], op=ALU.mult), [pq, t.Di], [t.QK])
            for h in range(H):
                P.add("pe", (lambda h: (lambda e: e.transpose(out=t.ptr[:, h * CHK:(h + 1) * CHK], in_=t.XT[0][:, h, :], identity=c.identb[:, :])))(h), [t.XT[0], c.identb], [t.ptr])
            P.add("act", lambda e: e.activation(out=t.X[0][:, :, :], in_=t.ptr[:, 0:H * CHK], func=AF.Copy), [t.ptr], [t.X[0]])
        fs.append(f_kk)

        A_, AT_ = t.X[0], t.XT[0]
        Td, TdT = t.PT[0], t.PT[1]

        def f_base():
            P.add("pool", lambda e: e.tensor_tensor(out=t.IX[:, :, :], in0=A_[:, :, :], in1=bcm(c.gmask[:, 0, :], H), op=ALU.mult), [A_, c.gmask], [t.IX])
            P.add("pool", lambda e: e.tensor_tensor(out=Td[:, :, :], in0=bcm(c.identb[:, :], H), in1=t.IX[:, :, :], op=ALU.subtract), [t.IX, c.identb], [Td])
            P.add("pool", lambda e: e.tensor_tensor(out=t.IX[:, :, :], in0=AT_[:, :, :], in1=bcm(c.gmask[:, 0, :], H), op=ALU.mult), [AT_, c.gmask], [t.IX])
            P.add("pool", lambda e: e.tensor_tensor(out=TdT[:, :, :], in0=bcm(c.identb[:, :], H), in1=t.IX[:, :, :], op=ALU.subtract), [t.IX, c.identb], [TdT])
        fs.append(f_base)
        NMRG = 6

        def mk_merge(lv):
            def f():
                lastlv = (lv == NMRG)
                U, UT = t.U, t.UT
                if not lastlv:
                    pu = bank(t)
                    for h in range(H):
                        P.add("pe", (lambda h: (lambda e: e.matmul(pu[:, h * CHK:(h + 1) * CHK], lhsT=AT_[:, h, :], rhs=Td[:, h, :], start=True, stop=True)))(h), [AT_, Td], [pu])
                    P.add("dve", lambda e: e.tensor_tensor(out=U[:, :, :], in0=pu[:, :], in1=gm4[:, lv, :], op=ALU.mult), [pu, gm4], [U])
                put = bank(t)
                for h in range(H):
                    P.add("pe", (lambda h: (lambda e: e.matmul(put[:, h * CHK:(h + 1) * CHK], lhsT=A_[:, h, :], rhs=TdT[:, h, :], start=True, stop=True)))(h), [A_, TdT], [put])
                P.add("dve", lambda e: e.tensor_tensor(out=UT[:, :, :], in0=put[:, :], in1=gm4[:, lv, :], op=ALU.mult), [put, gm4], [UT])
                if not lastlv:
                    pw = bank(t)
                    for h in range(H):
                        P.add("pe", (lambda h: (lambda e: e.matmul(pw[:, h * CHK:(h + 1) * CHK], lhsT=TdT[:, h, :], rhs=c.identb[:, :], start=True, stop=False)))(h), [TdT, c.identb], [pw])
                        P.add("pe", (lambda h: (lambda e: e.matmul(pw[:, h * CHK:(h + 1) * CHK], lhsT=TdT[:, h, :], rhs=U[:, h, :], start=False, stop=True)))(h), [TdT, U], [pw])
                pwt = bank(t)
                for h in range(H):
                    P.add("pe", (lambda h: (lambda e: e.matmul(pwt[:, h * CHK:(h + 1) * CHK], lhsT=Td[:, h, :], rhs=c.identb[:, :], start=True, stop=False)))(h), [Td, c.identb], [pwt])
                    P.add("pe", (lambda h: (lambda e: e.matmul(pwt[:, h * CHK:(h + 1) * CHK], lhsT=Td[:, h, :], rhs=UT[:, h, :], start=False, stop=True)))(h), [Td, UT], [pwt])
                if not lastlv:
                    P.add("act", lambda e: e.activation(out=Td[:, :, :], in_=pw[:, :], func=AF.Copy), [pw], [Td])
                P.add("act", lambda e: e.activation(out=TdT[:, :, :], in_=pwt[:, :], func=AF.Copy), [pwt], [TdT])
            return f
        for lv in range(1, NMRG + 1):
            fs.append(mk_merge(lv))
        PTf = TdT
        egc = t.E12[:, 0:4]

        def f_scan1():
            p = bank(t)
            for h in range(H):
                P.add("pe", (lambda h: (lambda e: e.matmul(p[:, h * 64:(h + 1) * 64], lhsT=kT[:, h, :], rhs=t.Sb[:, h, :], start=True, stop=True)))(h), [kT, t.Sb], [p])
            P.add("dve", lambda e: e.tensor_tensor(out=t.t2[:, :, :], in0=p[:, 0:H * 64], in1=bc3(egc, H, 64), op=ALU.mult), [p, t.E12], [t.t2])
            P.add("pool", lambda e: e.tensor_tensor(out=t.rb[:, :, :], in0=vt[:, :, :], in1=t.t2[:, :, :], op=ALU.subtract), [vt, t.t2], [t.rb])
            P.add("pool", lambda e: e.tensor_tensor(out=t.kd[:, :, :], in0=kt[:, :, :], in1=bc3(t.E12[:, 4:8], H, 64), op=ALU.mult), [kt, t.E12], [t.kd])
        fs.append(f_scan1)

        def f_scan2():
            p = bank(t)
            for h in range(H):
                P.add("pe", (lambda h: (lambda e: e.matmul(p[:, h * 64:(h + 1) * 64], lhsT=PTf[:, h, :], rhs=t.rb[:, h, :], start=True, stop=True)))(h), [PTf, t.rb], [p])
            P.add("dve", lambda e: e.tensor_tensor(out=t.vn[:, :, :], in0=p[:, 0:H * 64], in1=bc3(be, H, 64), op=ALU.mult), [p, t.g], [t.vn])
        fs.append(f_scan2)

        def f_scan3():
            p1 = bank(t)
            for h in range(H):
                P.add("pe", (lambda h: (lambda e: e.matmul(p1[:, h * 64:(h + 1) * 64], lhsT=qT[:, h, :], rhs=t.Sb[:, h, :], start=True, stop=True)))(h), [qT, t.Sb], [p1])
            o = t.o[si % 2]
            P.add("dve", lambda e: e.tensor_tensor(out=t.t2[:, :, :], in0=p1[:, 0:H * 64], in1=bc3(egc, H, 64), op=ALU.mult), [p1, t.E12], [t.t2])
            p2 = bank(t)
            for h in range(H):
                P.add("pe", (lambda h: (lambda e: e.matmul(p2[:, h * 64:(h + 1) * 64], lhsT=t.QK[:, h, :], rhs=t.vn[:, h, :], start=True, stop=True)))(h), [t.QK, t.vn], [p2])
            P.add("dve", lambda e: e.tensor_tensor(out=o[:, :, :], in0=p2[:, 0:H * 64], in1=t.t2[:, :, :], op=ALU.add), [p2, t.t2], [o])
            P.dma("sp", m.OG[d, t.b, t0:t0 + CHK, :].rearrange("p (h d) -> p h d", d=64), o[:, :, :], [o], [P.R(("OG", d, t.b, t0))], ("so", t.b, t.d, si % 2))
            p3 = bank(t)
            for h in range(H):
                P.add("pe", (lambda h: (lambda e: e.matmul(p3[0:64, h * 64:(h + 1) * 64], lhsT=t.kd[:, h, :], rhs=t.vn[:, h, :], start=True, stop=True)))(h), [t.kd, t.vn], [p3])
            P.add("pool", lambda e: e.tensor_tensor(out=t.t4[:, :, :], in0=t.S[:, :, :], in1=bc3(t.E12[0:64, 8:12], H, 64), op=ALU.mult), [t.S, t.E12], [t.t4])
            P.add("dve", lambda e: e.tensor_tensor(out=t.S[:, :, :], in0=p3[0:64, 0:H * 64], in1=t.t4[:, :, :], op=ALU.add), [p3, t.t4], [t.S])
            P.add("act", lambda e: e.activation(out=t.Sb[:, :, :], in_=t.S[:, :, :], func=AF.Copy), [t.S], [t.Sb])
        fs.append(f_scan3)
        return fs

    nst = (T // CHK) if GDN_STEPS is None else GDN_STEPS
    for t in th:
        load(t, 0)
    for si in range(nst):
        for t in th:
            if si + 1 < nst:
                load(t, si + 1)
        fss = [step(t, si)[:GSUB] for t in th]
        for grp in zip(*fss):
            for f_ in grp:
                f_()


def stage_gdn_fin(m, l, last):
    P, c = m.P, m.c
    tl = [(b, t0, Tt) for b in range(2) for (t0, Tt) in tiles_of_batch() if not (last and t0 < CT)]
    of = [P.sb(f"nof{i}", [128, 4, 256], F32) for i in range(2)]
    ob = [P.sb(f"nob{i}", [128, 4, 256], F32) for i in range(2)]
    zz = [P.sb(f"nzz{i}", [128, 4, 256], F32) for i in range(2)]
    sq = P.sb("nsq", [128, 4, 256], F32)
    ss = P.sb("nss", [128, 16], F32)
    ya = P.sb("nya", [128, 4, 256], F32)
    st = P.sb("nst", [128, 2, 512], BF16)
    epsc = P.sb("nepsc", [128, 1], F32)
    P.add("pool", lambda e: e.memset(epsc[:, :], EPS), [], [epsc])
    ps = [P.ps(f"nps{i}", [128, 512]) for i in range(2)]

    def load(i):
        b, t0, Tt = tl[i]
        ns = Tt // 128
        k = i % 2
        P.dma("sp", of[k][:, 0:ns, :], m.OG[0, b, t0:t0 + Tt, :].rearrange("(n p) f -> p n f", p=128), [P.R(("OG", 0, b, t0 + n * 128)) for n in range(ns)], [of[k]], ("nof", k))
        P.dma("sp", ob[k][:, 0:ns, :], m.OG[1, b, t0:t0 + Tt, :].rearrange("(n p) f -> p n f", p=128), [P.R(("OG", 1, b, t0 + n * 128)) for n in range(ns)], [ob[k]], ("nob", k))
        P.dma("sp", zz[k][:, 0:ns, :], m.Zs[b, t0:t0 + Tt, :].rearrange("(n p) f -> p n f", p=128), [P.R(("Zs", b, t0))], [zz[k]], ("nzz", k))

    def body(i, b, t0, Tt):
        ns = Tt // 128
        k = i % 2
        o_, b_, z_ = of[k], ob[k], zz[k]
        P.add("dve", lambda e: e.tensor_tensor(out=o_[:, 0:ns, :], in0=o_[:, 0:ns, :], in1=b_[:, 0:ns, :], op=ALU.add), [o_, b_], [o_])
        P.add("pool", lambda e: e.tensor_tensor(out=sq[:, 0:ns, :], in0=o_[:, 0:ns, :], in1=o_[:, 0:ns, :], op=ALU.mult), [o_], [sq])
        P.add("dve", lambda e: e.tensor_reduce(out=ss[:, 0:ns * 4], in_=sq[:, 0:ns, :].rearrange("p n (h d) -> p (n h) d", d=64), axis=AX.X, op=ALU.add), [sq], [ss])
        P.add("act", lambda e: e.activation(out=ss[:, 0:ns * 4], in_=ss[:, 0:ns * 4], func=AF.Sqrt, scale=1.0 / 64, bias=epsc[:, 0:1]), [ss, epsc], [ss])
        P.add("dve", lambda e: e.reciprocal(out=ss[:, 0:ns * 4], in_=ss[:, 0:ns * 4]), [ss], [ss])
        ov = o_[:, 0:ns, :].rearrange("p n (h d) -> p (n h) d", d=64)
        yv = ya[:, 0:ns, :].rearrange("p n (h d) -> p (n h) d", d=64)
        P.add("dve", lambda e: e.tensor_tensor(out=yv, in0=ov, in1=bc3(ss[:, 0:ns * 4], ns * 4, 64), op=ALU.mult), [o_, ss], [ya])
        P.add("pool", lambda e: e.tensor_tensor(out=yv, in0=yv, in1=bcm(c.gdng[:, l, :], ns * 4), op=ALU.mult), [ya, c.gdng], [ya])
        P.add("dve", lambda e: e.tensor_tensor(out=ya[:, 0:ns, :], in0=ya[:, 0:ns, :], in1=z_[:, 0:ns, :], op=ALU.mult), [ya, z_], [ya])
        for j in range(2):
            for n in range(ns):
                P.add("pe", (lambda j, n: (lambda e: e.transpose(out=ps[j][:, n * 128:(n + 1) * 128], in_=ya[:, n, j * 128:(j + 1) * 128], identity=c.ident[:, :])))(j, n), [ya, c.ident], [ps[j]])
            if j == 0:
                P.add("act", lambda e: e.activation(out=st[:, 0, 0:Tt], in_=ps[0][:, 0:Tt], func=AF.Copy), [ps[0]], [st])
            else:
                P.add("dve", lambda e: e.tensor_copy(out=st[:, 1, 0:Tt], in_=ps[1][:, 0:Tt]), [ps[1]], [st])
        P.dma("sp", m.YT[b][0:256, :].rearrange("(j p) t -> p j t", p=128)[:, :, t0:t0 + Tt], st[:, :, 0:Tt], [st], [P.R(("YT", b, t0))], "nst")

    load(0)
    for i, (b, t0, Tt) in enumerate(tl):
        if i + 1 < len(tl):
            load(i + 1)
        body(i, b, t0, Tt)
```

```python
import numpy as np
import concourse.bass as bass
import concourse.mybir as mybir
from contextlib import ExitStack

F32 = mybir.dt.float32
BF16 = mybir.dt.bfloat16
AF = mybir.ActivationFunctionType
ALU = mybir.AluOpType
AX = mybir.AxisListType

ENGS = ("pe", "act", "dve", "pool", "sp")
KROT = 4


class Res:
    __slots__ = ("name", "w", "rd")

    def __init__(self, name=""):
        self.name = name
        self.w = None
        self.rd = []


class Ins:
    __slots__ = ("eng", "fn", "deps", "dma", "tok", "marked", "idx", "ph")

    def __init__(self, eng, fn, dma):
        self.eng = eng
        self.fn = fn
        self.deps = []
        self.dma = dma
        self.tok = None
        self.marked = False


class Buf:
    def __init__(self, t, res):
        self.t = t
        self.res = res

    def __getitem__(self, k):
        return self.t[k]


class Prog:
    def __init__(self, nc):
        self.nc = nc
        self.q = {e: [] for e in ENGS}
        self.es = ExitStack()
        self.dma_sems = {}
        self.dma_cnt = {}
        self.nbuf = 0
        self.dram_res = {}

    def sb(self, name, shape, dtype):
        self.nbuf += 1
        name = f"{name}_u{self.nbuf}"
        t = self.es.enter_context(self.nc.sbuf_tensor(name, list(shape), dtype))
        return Buf(t, Res(name))

    def ps(self, name, shape, dtype=F32):
        self.nbuf += 1
        name = f"{name}_u{self.nbuf}"
        t = self.es.enter_context(self.nc.psum_tensor(name, list(shape), dtype))
        return Buf(t, Res(name))

    def dram(self, name, shape, dtype, kind=None):
        if kind is None:
            t = self.nc.dram_tensor(name, list(shape), dtype)
        else:
            t = self.nc.dram_tensor(name, list(shape), dtype, kind=kind)
        return t.ap()

    def R(self, key):
        r = self.dram_res.get(key)
        if r is None:
            r = Res(str(key))
            self.dram_res[key] = r
        return r

    def add(self, eng, fn, reads=(), writes=(), dma=None):
        if dma is not None:
            km = self.__dict__.setdefault("keymap", {})
            dma = ("kp", km.setdefault(dma, len(km)))
        I = Ins(eng, fn, dma)
        I.ph = getattr(self, "phase", 0)
        deps = []
        for r in reads:
            r = r.res if isinstance(r, Buf) else r
            if r.w is not None:
                deps.append(r.w)
        for w in writes:
            w = w.res if isinstance(w, Buf) else w
            deps.extend(w.rd)
            if w.w is not None:
                deps.append(w.w)
        for d in deps:
            if d is I or d.ph != I.ph:
                continue
            if d.eng == "pe" and eng == "pe" and d.dma is None and dma is None:
                continue
            I.deps.append(d)
            d.marked = True
        for r in reads:
            r = r.res if isinstance(r, Buf) else r
            r.rd.append(I)
        for w in writes:
            w = w.res if isinstance(w, Buf) else w
            w.w = I
            w.rd = []
        self.q[eng].append(I)
        return I

    def dma(self, eng, out, in_, reads, writes, key, **kw):
        return self.add(eng, lambda e: e.dma_start(out=out, in_=in_, **kw), reads, writes, dma=key)

    def emit(self):
        nc = self.nc
        es = self.es
        esem = {e: [es.enter_context(nc.semaphore(f"s_{e}{k}")) for k in range(KROT)] for e in ENGS}
        for e in ENGS:
            m = 0
            for I in self.q[e]:
                if I.dma is not None:
                    key = I.dma
                    if key not in self.dma_sems:
                        self.dma_sems[key] = es.enter_context(nc.semaphore(f"d_{len(self.dma_sems)}"))
                        self.dma_cnt[key] = 0
                    self.dma_cnt[key] += 1
                    I.tok = (self.dma_sems[key], 16 * self.dma_cnt[key])
                elif I.marked:
                    I.tok = (esem[e][m % KROT], m // KROT + 1)
                    m += 1
        block = es.enter_context(nc.Block())
        stats = {}

        def run(eng_name, eh):
            waited = {}
            nw = 0
            for I in self.q[eng_name]:
                need = {}
                for d in I.deps:
                    s, v = d.tok
                    k = id(s)
                    if waited.get(k, 0) >= v:
                        continue
                    if k not in need or need[k][1] < v:
                        need[k] = (s, v)
                for k, (s, v) in need.items():
                    eh.wait_ge(s, v)
                    waited[k] = v
                    nw += 1
                h = I.fn(eh)
                if I.tok is not None:
                    if I.dma is not None:
                        h.then_inc(I.tok[0], 16)
                    else:
                        h.then_inc(I.tok[0], 1)
            stats[eng_name] = (len(self.q[eng_name]), nw)

        @block.tensor
        def _(e):
            run("pe", e)

        @block.scalar
        def _(e):
            run("act", e)

        @block.vector
        def _(e):
            run("dve", e)

        @block.gpsimd
        def _(e):
            run("pool", e)

        @block.sync
        def _(e):
            run("sp", e)

        self.stats = stats
        return stats

    def finish_wait(self, eng, instrs):
        I = Ins(eng, lambda e: e.nop(), None)
        for d in instrs:
            I.deps.append(d)
            d.marked = True
        self.q[eng].append(I)
        return I


def _phase_begin(self):
    self.pes = ExitStack()
    self._glob_es = self.es
    self.es = self.pes
    self.q = {e: [] for e in ENGS}
    self.phase = getattr(self, "phase", 0) + 1
    self.keymap = {}


def _phase_end(self):
    lasts = []
    for e in ENGS:
        for I in reversed(self.q[e]):
            if I.dma is None:
                lasts.append(I)
                break
    dmas = [I for e in ENGS for I in self.q[e] if I.dma is not None]
    for e in ENGS:
        self.finish_wait(e, lasts + dmas)
    self._emit_block()
    self.es = self._glob_es
    self.pes.close()
    for r in self.dram_res.values():
        r.w = None
        r.rd = []


def _emit_block(self):
    nc = self.nc
    ges = self._glob_es
    if not hasattr(self, "esem"):
        self.esem = {e: [ges.enter_context(nc.semaphore(f"s_{e}{k}")) for k in range(KROT)] for e in ENGS}
        self.ecnt = {e: 0 for e in ENGS}
        self.waited = {e: {} for e in ENGS}
        self.tot = {e: [0, 0] for e in ENGS}
    for e in ENGS:
        for I in self.q[e]:
            if I.dma is not None:
                key = I.dma
                if key not in self.dma_sems:
                    self.dma_sems[key] = ges.enter_context(nc.semaphore(f"d_{len(self.dma_sems)}"))
                    self.dma_cnt[key] = 0
                self.dma_cnt[key] += 1
                I.tok = (self.dma_sems[key], 16 * self.dma_cnt[key])
            elif I.marked:
                m = self.ecnt[e]
                I.tok = (self.esem[e][m % KROT], m // KROT + 1)
                self.ecnt[e] = m + 1
    with nc.Block() as block:
        def run(eng_name, eh):
            waited = self.waited[eng_name]
            for I in self.q[eng_name]:
                need = {}
                for d in I.deps:
                    s, v = d.tok
                    k = id(s)
                    if waited.get(k, 0) >= v:
                        continue
                    if k not in need or need[k][1] < v:
                        need[k] = (s, v)
                for k, (s, v) in need.items():
                    eh.wait_ge(s, v)
                    waited[k] = v
                    self.tot[eng_name][1] += 1
                h = I.fn(eh)
                self.tot[eng_name][0] += 1
                if I.tok is not None:
                    h.then_inc(I.tok[0], 16 if I.dma is not None else 1)

        @block.tensor
        def _(e):
            run("pe", e)

        @block.scalar
        def _(e):
            run("act", e)

        @block.vector
        def _(e):
            run("dve", e)

        @block.gpsimd
        def _(e):
            run("pool", e)

        @block.sync
        def _(e):
            run("sp", e)


Prog.phase_begin = _phase_begin
Prog.phase_end = _phase_end
Prog._emit_block = _emit_block
from concourse.bass_utils import run_bass_kernel_spmd

D = 1024
L = 4096
CT = 256
T = L + CT
TP = T + 4
NL = 4
DFF = 2816
DFE = 1408
NE = 8
EPS = 1e-6
NFM = 24
NTM = 400
NCOLS = NFM * 128 + NTM
R_N1, R_N2, R_BM, R_CA, R_CC, R_QG, R_QGP, R_KG, R_KGP, R_NF, NVR = 0, 8, 16, 64, 82, 88, 89, 90, 91, 92, 100
CHK = 128
NOCAST = False
GDN_STEPS = None
GSUB = 99
IMPLEMENTED_FFN = True
SUB = 9
C0SHIFT = 0
SEC = 9
NTILES = 99


def pc(t):
    return t + 1 if t < CT else t + 3


def tiles_of_batch():
    return [(0, CT)] + [(CT + 512 * i, 512) for i in range(L // 512)]


class M:
    pass


def build(nlayers=NL, debug=(), stop=None, skip=()):
    nc = bass.Bass("TRN2", target_bir_lowering=False)
    P = Prog(nc)
    m = M()
    m.nc, m.P = nc, P
    m.nocast = NOCAST
    dbg = set(debug)

    def scratch(name, shape, dt):
        return P.dram(name, shape, dt, kind="ExternalOutput" if name in dbg else None)

    I_ = lambda n, s, dt=F32: P.dram(n, s, dt, kind="ExternalInput")
    m.x_in = I_("x", [2, L, D])
    m.ctx_in = I_("ctx", [2, CT, D])
    m.cvec = I_("cvec", [4, D])
    m.w_mod = I_("w_mod", [NL, D, 6 * D])
    m.vecs = I_("vecs", [NL, NVR, 128])
    m.gdn_g = I_("gdn_g", [NL, 64])
    m.gatec = I_("gatec", [NL, 16])
    m.w_in = I_("w_in", [NL, D, NCOLS])
    m.w_out = I_("w_out", [NL, D, D])
    m.ffn_g = I_("ffn_g", [2, D, DFF])
    m.ffn_u = I_("ffn_u", [2, D, DFF])
    m.ffn_d = I_("ffn_d", [2, DFF, D])
    m.router = I_("router", [2, D, NE])
    m.moe_g = I_("moe_g", [2, NE, D, DFE])
    m.moe_u = I_("moe_u", [2, NE, D, DFE])
    m.moe_d = I_("moe_d", [2, NE, DFE, D])
    m.ropeC = I_("ropeC", [128, T])
    m.ropeS = I_("ropeS", [128, T])
    m.gmask_in = I_("gmask", [128, 7, 128])
    m.out = P.dram("out", [2, L, D], F32, kind="ExternalOutput")
    m.w_in_b = P.dram("w_in_b", [NL, D, NCOLS], BF16)
    m.w_out_b = P.dram("w_out_b", [NL, D, D], BF16)
    m.ffn_g_b = P.dram("ffn_g_b", [2, D, DFF], BF16)
    m.ffn_u_b = P.dram("ffn_u_b", [2, D, DFF], BF16)
    m.ffn_d_b = P.dram("ffn_d_b", [2, DFF, D], BF16)
    m.moe_g_b = P.dram("moe_g_b", [2, NE, D, DFE], BF16)
    m.moe_u_b = P.dram("moe_u_b", [2, NE, D, DFE], BF16)
    m.moe_d_b = P.dram("moe_d_b", [2, NE, DFE, D], BF16)
    m.XT = scratch("XT", [2, D, T], F32)
    m.QKVp = scratch("QKVp", [2, 768, TP], F32)
    m.Zs = scratch("Zs", [2, T, 256], F32)
    m.G = scratch("G", [2, 16, T], F32)
    m.QA = scratch("QA", [2, 512, T], BF16)
    m.KA = scratch("KA", [2, 2, 128, T], BF16)
    m.VA = scratch("VA", [2, T, 128], BF16)
    m.CB = scratch("CB", [2, 256, T], F32)
    m.PP = scratch("PP", [2, 256, TP], F32)
    m.QG = scratch("QG", [2, 256, T], BF16)
    m.KG = scratch("KG", [2, 256, T], BF16)
    m.KGt = scratch("KGt", [2, T, 256], F32)
    m.VGt = scratch("VGt", [2, T, 256], F32)
    m.OG = scratch("OG", [2, 2, T, 256], F32)
    m.YT = scratch("YT", [2, D, T], BF16)
    m.debug = dbg

    c = M()
    m.c = c
    c.ident = P.sb("ident", [128, 128], F32)
    c.identb = P.sb("identb", [128, 128], BF16)
    c.onesb = P.sb("onesb", [128, 128], BF16)
    c.bdiag = P.sb("bdiag", [128, 128], BF16)
    c.onesf = P.sb("onesf", [128, 128], F32)
    c.vcol = P.sb("vcol", [128, NL, NVR], F32)
    c.modv = P.sb("modv", [128, NL, 48, 4], F32)
    c.gm1 = P.sb("gm1", [128, NL, 8, 4], F32)
    c.gm2 = P.sb("gm2", [128, NL, 8, 4], F32)
    c.gatec = P.sb("gatecs", [128, NL, 16], F32)
    c.gdng = P.sb("gdng", [128, NL, 64], F32)
    c.zero = P.sb("zero", [128, 512], F32)
    m.gcol = P.sb("gcol", [8, NL, 2], F32)
    c.sel = P.sb("sel", [8, 8, 128], F32)
    c.gmask = P.sb("gmaskc", [128, 7, 128], F32)
    c.Ui = P.sb("Ui", [128, 128], F32)
    c.Us = P.sb("Us", [128, 128], F32)
    c.Li = P.sb("Li", [128, 128], F32)
    c.Ls = P.sb("Ls", [128, 128], F32)
    c.zerob = P.sb("zerob", [128, 2176], BF16)
    m.skip = set(skip)

    P.phase_begin()
    prologue(m)
    P.phase_end()
    if stop == "pro":
        return finish(m)
    for l in range(nlayers):
        last = (l == NL - 1)
        P.phase_begin(); stage_proj(m, l); P.phase_end()
        if stop == ("s1", l): return finish(m)
        if 'gdn' in skip:
            P.phase_begin(); stage_attn(m, l, last); P.phase_end()
            if stop == ('at', l): return finish(m)
            P.phase_begin(); stage_ffn(m, l, last); P.phase_end()
            if stop == ('ff', l): return finish(m)
            continue
        P.phase_begin(); stage_gdn_pre(m, l); P.phase_end()
        if stop == ("g1", l): return finish(m)
        P.phase_begin(); stage_gdn_scan(m, l); P.phase_end()
        if stop == ("g2", l): return finish(m)
        P.phase_begin(); stage_gdn_fin(m, l, last); P.phase_end()
        if stop == ("g3", l): return finish(m)
        P.phase_begin(); stage_attn(m, l, last); P.phase_end()
        if stop == ("at", l): return finish(m)
        P.phase_begin(); stage_ffn(m, l, last); P.phase_end()
        if stop == ("ff", l): return finish(m)
    P.phase_begin(); epilogue(m); P.phase_end()
    return finish(m)


def finish(m):
    m.P._glob_es = m.P.es
    m.P.es.close()
    return m


def cast_dma(P, dst, src, key, rows):
    n = src.shape[0]
    for r0 in range(0, n, rows):
        r1 = min(n, r0 + rows)
        P.add("pool", (lambda o, i: (lambda e: e.dma_start(out=o, in_=i, max_dma_last_dim=4096)))(dst[r0:r1, :], src[r0:r1, :]),
              [], [P.R(key)], dma="cast")


def casts_proj(m, l):
    P = m.P
    cast_dma(P, m.w_in_b[l], m.w_in[l], ("w_in_b", l), 512)
    cast_dma(P, m.w_out_b[l], m.w_out[l], ("w_out_b", l), 512)


def casts_ffn(m, l):
    P = m.P
    i = l // 2
    if l % 2 == 0:
        cast_dma(P, m.ffn_g_b[i], m.ffn_g[i], ("ffn_g_b", i), 512)
        cast_dma(P, m.ffn_u_b[i], m.ffn_u[i], ("ffn_u_b", i), 512)
        cast_dma(P, m.ffn_d_b[i], m.ffn_d[i], ("ffn_d_b", i), 704)
    else:
        for e in range(NE):
            cast_dma(P, m.moe_g_b[i, e], m.moe_g[i, e], ("moe_g_b", i, e), 512)
            cast_dma(P, m.moe_u_b[i, e], m.moe_u[i, e], ("moe_u_b", i, e), 512)
            cast_dma(P, m.moe_d_b[i, e], m.moe_d[i, e], ("moe_d_b", i, e), 704)


def prologue(m):
    P, c = m.P, m.c
    P.add("pool", lambda e: e.memset(c.ident[:, :], 1.0), [], [c.ident])
    P.add("pool", lambda e: e.affine_select(out=c.ident[:, :], in_=c.ident[:, :], pattern=[[-1, 128]],
                                            compare_op=ALU.is_equal, fill=0.0, base=0, channel_multiplier=1),
          [c.ident], [c.ident])
    P.add("pool", lambda e: e.tensor_copy(out=c.identb[:, :], in_=c.ident[:, :]), [c.ident], [c.identb])
    P.add("pool", lambda e: e.memset(c.onesb[:, :], 1.0), [], [c.onesb])
    P.add("pool", lambda e: e.memset(c.onesf[:, :], 1.0), [], [c.onesf])
    P.add("pool", lambda e: e.memset(c.zero[:, :], 0.0), [], [c.zero])
    P.add("pool", lambda e: e.memset(c.bdiag[:, :], 0.0), [], [c.bdiag])
    P.add("pool", lambda e: e.memset(c.bdiag[0:64, 0:64], 1.0), [], [c.bdiag])
    P.add("pool", lambda e: e.memset(c.bdiag[64:128, 64:128], 1.0), [], [c.bdiag])
    P.add("pool", lambda e: e.tensor_copy(out=c.sel[:, :, :], in_=c.ident[0:8, 0:8].unsqueeze(2).to_broadcast([8, 8, 128])), [c.ident], [c.sel])
    for (mk, coef, cm, op) in ((c.Ui, 1, -1, ALU.is_ge), (c.Us, 1, -1, ALU.is_gt), (c.Li, -1, 1, ALU.is_ge), (c.Ls, -1, 1, ALU.is_gt)):
        P.add("pool", (lambda mk: (lambda e: e.memset(mk[:, :], 1.0)))(mk), [], [mk])
        P.add("pool", (lambda mk, coef, cm, op: (lambda e: e.affine_select(out=mk[:, :], in_=mk[:, :], pattern=[[coef, 128]], compare_op=op, fill=0.0, base=0, channel_multiplier=cm)))(mk, coef, cm, op), [mk], [mk])
    casts_proj(m, 0)
    if "gdn" in m.skip:
        P.add("pool", lambda e: e.memset(c.zerob[:, :], 0.0), [], [c.zerob])
        for b in range(2):
            for hf_ in range(2):
                for cc_ in range(2):
                    P.dma("sp", m.YT[b][cc_ * 128:(cc_ + 1) * 128, hf_ * 2176:(hf_ + 1) * 2176], c.zerob[:, :], [c.zerob], [P.R(("YTz", b, hf_, cc_))], "padz")
    for b in range(2):
        for (dst, nr) in ((m.QKVp, 768), (m.PP, 256)):
            v = dst[b].rearrange("(c p) t -> p c t", p=128)
            ncn = nr // 128
            for (a0, a1) in ((0, 1), (CT + 1, CT + 3), (TP - 1, TP)):
                P.dma("sp", v[:, :, a0:a1], c.zero[:, 0:ncn * (a1 - a0)].rearrange("p (c t) -> p c t", c=ncn),
                      [c.zero], [P.R(("pad", b, nr, a0))], "padz", allow_slow_non_contiguous=True)
    ps = [P.ps(f"pps{i}", [128, 512]) for i in range(4)]
    for l in range(NL):
        vt = P.sb(f"vt{l}", [NVR, 128], F32)
        P.dma("sp", vt[:, :], m.vecs[l], [], [vt], ("vt", l))
        P.add("pe", (lambda l, vt: (lambda e: e.transpose(out=ps[0][:, l * NVR:(l + 1) * NVR], in_=vt[:, :], identity=c.ident[0:NVR, 0:NVR])))(l, vt),
              [vt, c.ident], [ps[0]])
    P.add("dve", lambda e: e.tensor_copy(out=c.vcol[:, :, :], in_=ps[0][:, 0:NL * NVR].rearrange("p (l r) -> p l r", l=NL)), [ps[0]], [c.vcol])
    P.dma("sp", c.gmask[:, :, :], m.gmask_in, [], [c.gmask], "gmaskc")
    P.dma("sp", c.gatec[:, :, :], m.gatec.partition_broadcast(128), [], [c.gatec], "gatec")
    P.dma("sp", c.gdng[:, :, :], m.gdn_g.partition_broadcast(128), [], [c.gdng], "gdng")
    P.dma("sp", m.gcol[:, :, :], m.gatec.rearrange("l (j p) -> p l j", p=8), [], [m.gcol], "gcol", allow_slow_non_contiguous=True)
    P.add("act", lambda e: e.activation(out=m.gcol[:, :, 1:2], in_=m.gcol[:, :, 1:2], func=AF.Exp), [m.gcol], [m.gcol])
    P.add("dve", lambda e: e.tensor_scalar(out=m.gcol[:, :, 1:2], in0=m.gcol[:, :, 1:2], scalar1=-1.0, scalar2=None, op0=ALU.mult), [m.gcol], [m.gcol])
    P.add("act", lambda e: e.activation(out=c.gatec[:, :, 8:16], in_=c.gatec[:, :, 8:16], func=AF.Exp), [c.gatec], [c.gatec])
    P.add("dve", lambda e: e.tensor_scalar(out=c.gatec[:, :, 8:16], in0=c.gatec[:, :, 8:16], scalar1=-1.0, scalar2=None, op0=ALU.mult), [c.gatec], [c.gatec])
    cv = P.sb("cv", [4, D], F32)
    P.dma("sp", cv[:, :], m.cvec, [], [cv], "cv")
    P.add("act", lambda e: e.activation(out=cv[:, :], in_=cv[:, :], func=AF.Silu), [cv], [cv])
    sT = P.sb("sT", [128, 8, 4], F32)
    for kc in range(8):
        P.add("pe", (lambda kc: (lambda e: e.transpose(out=ps[1][:, kc * 4:kc * 4 + 4], in_=cv[:, kc * 128:(kc + 1) * 128], identity=c.ident[0:4, 0:4])))(kc),
              [cv, c.ident], [ps[1]])
    P.add("dve", lambda e: e.tensor_copy(out=sT[:, :, :], in_=ps[1][:, 0:32].rearrange("p (k w) -> p k w", k=8)), [ps[1]], [sT])
    wm = [P.sb(f"wm{i}", [128, 8, 512], F32) for i in range(2)]
    it = 0
    for l in range(NL):
        pm = ps[2 + (l % 2)]
        for g in range(12):
            w = wm[it % 2]
            it += 1
            P.dma("sp", w[:, :, :], m.w_mod[l][:, g * 512:(g + 1) * 512].rearrange("(k p) n -> p k n", p=128), [], [w], ("wm", it % 2))
            for j4 in range(4):
                j = g * 4 + j4
                for kc in range(8):
                    P.add("pe", (lambda w, j, j4, kc, pm: (lambda e: e.matmul(pm[:, j * 4:j * 4 + 4], lhsT=w[:, kc, j4 * 128:(j4 + 1) * 128], rhs=sT[:, kc, :],
                                                                               start=(kc == 0), stop=(kc == 7))))(w, j, j4, kc, pm),
                          [w, sT], [pm])
        P.add("dve", (lambda l, pm: (lambda e: e.tensor_tensor(out=c.modv[:, l, :, :], in0=pm[:, 0:192].rearrange("p (j w) -> p j w", w=4),
                                                                in1=c.vcol[:, l, R_BM:R_BM + 48].unsqueeze(2).to_broadcast([128, 48, 4]), op=ALU.add)))(l, pm),
              [pm, c.vcol], [c.modv])
        P.add("dve", (lambda l: (lambda e: e.scalar_tensor_tensor(out=c.gm1[:, l, :, :], in0=c.modv[:, l, 8:16, :], scalar=1.0,
                                                                  in1=c.vcol[:, l, R_N1:R_N1 + 8].unsqueeze(2).to_broadcast([128, 8, 4]), op0=ALU.add, op1=ALU.mult)))(l),
              [c.modv, c.vcol], [c.gm1])
        P.add("dve", (lambda l: (lambda e: e.scalar_tensor_tensor(out=c.gm2[:, l, :, :], in0=c.modv[:, l, 32:40, :], scalar=1.0,
                                                                  in1=c.vcol[:, l, R_N2:R_N2 + 8].unsqueeze(2).to_broadcast([128, 8, 4]), op0=ALU.add, op1=ALU.mult)))(l),
              [c.modv, c.vcol], [c.gm2])
    xin = [P.sb(f"xin{i}", [128, 4, D], F32) for i in range(2)]
    xo = [P.sb(f"xo{i}", [128, 8, 512], F32) for i in range(2)]
    pt = [P.ps(f"ppt{i}", [128, 512]) for i in range(4)]
    it = 0
    for b in range(2):
        for (t0, Tt) in tiles_of_batch():
            nb = Tt // 128
            xi, xout = xin[it % 2], xo[it % 2]
            it += 1
            src = m.ctx_in[b] if t0 < CT else m.x_in[b, t0 - CT:t0 - CT + Tt, :]
            P.dma("sp", xi[:, 0:nb, :], src.rearrange("(n p) d -> p n d", p=128), [], [xi], ("xin", it % 2))
            for cc in range(8):
                pp = pt[cc % 4]
                for n in range(nb):
                    P.add("pe", (lambda pp, xi, n, cc: (lambda e: e.transpose(out=pp[:, n * 128:(n + 1) * 128], in_=xi[:, n, cc * 128:(cc + 1) * 128], identity=c.ident[:, :])))(pp, xi, n, cc),
                          [xi, c.ident], [pp])
                eng = "dve" if cc % 2 == 0 else "act"
                if eng == "dve":
                    P.add("dve", (lambda pp, xout, cc, Tt: (lambda e: e.tensor_copy(out=xout[:, cc, 0:Tt], in_=pp[:, 0:Tt])))(pp, xout, cc, Tt), [pp], [xout])
                else:
                    P.add("act", (lambda pp, xout, cc, Tt: (lambda e: e.activation(out=xout[:, cc, 0:Tt], in_=pp[:, 0:Tt], func=AF.Copy)))(pp, xout, cc, Tt), [pp], [xout])
            P.dma("sp", m.XT[b].rearrange("(c p) t -> p c t", p=128)[:, :, t0:t0 + Tt], xout[:, :, 0:Tt], [xout], [P.R(("XT", b, t0))], ("xo", it % 2))


def _perm64():
    p = np.zeros(64, np.int64)
    for d in range(64):
        p[d] = d + 16 if (d % 32) < 16 else d - 16
    return p


def host_prep(inp):
    f = lambda a: np.ascontiguousarray(np.asarray(a, dtype=np.float32))
    w_in = f(inp["w_in"])
    p64 = _perm64()
    o_qkv, o_z, o_a, o_b, o_q, o_k, o_v, o_cb, o_cc, o_ch = 0, 768, 1024, 1032, 1040, 1552, 1680, 1808, 2064, 2320
    cols = []
    cols += list(range(o_qkv, o_qkv + 768))
    cols += list(range(o_q, o_q + 512))
    for h in range(8):
        cols += list(o_q + h * 64 + p64)
    k0 = list(range(o_k, o_k + 64)); k1 = list(range(o_k + 64, o_k + 128))
    k0p = list(o_k + p64); k1p = list(o_k + 64 + p64)
    cols += k0 + k1 + k1 + k0 + k0p + k1p + k1p + k0p
    cols += list(range(o_cb, o_cb + 768))
    cols += list(range(o_z, o_z + 256)) + list(range(o_a, o_a + 16)) + list(range(o_v, o_v + 128))
    cols = np.array(cols)
    assert cols.size == NCOLS
    w_in_ext = np.ascontiguousarray(w_in[:, :, cols])
    vecs = np.zeros((NL, NVR, 128), np.float32)
    qn, kn = f(inp["q_norm"]), f(inp["k_norm"])
    for l in range(NL):
        vecs[l, R_N1:R_N1 + 8] = f(inp["norm1"])[l].reshape(8, 128)
        vecs[l, R_N2:R_N2 + 8] = f(inp["norm2"])[l].reshape(8, 128)
        vecs[l, R_BM:R_BM + 48] = f(inp["b_mod"])[l].reshape(48, 128)
        vecs[l, R_CA:R_CA + 18] = f(inp["conv_a"])[l].reshape(18, 128)
        vecs[l, R_CC:R_CC + 6] = f(inp["conv_c"])[l].reshape(6, 128)
        vecs[l, R_QG] = np.tile(qn[l], 2)
        vecs[l, R_QGP] = np.tile(qn[l][p64], 2)
        vecs[l, R_KG] = np.tile(kn[l], 2)
        vecs[l, R_KGP] = np.tile(kn[l][p64], 2)
        vecs[l, R_NF:R_NF + 8] = f(inp["norm_f"]).reshape(8, 128)
    gatec = np.concatenate([f(inp["dt_bias"]).reshape(NL, 8), f(inp["a_log"]).reshape(NL, 8)], axis=1)
    quarter = 16
    inv_freq = (10000.0 ** (-np.arange(quarter, dtype=np.float32) / quarter)).astype(np.float32)
    t = np.arange(L)
    row, col = (t // 64).astype(np.float32), (t % 64).astype(np.float32)
    C = np.ones((64, T), np.float32); S = np.zeros((64, T), np.float32)
    for d in range(64):
        pos = row if d < 32 else col
        fi = d % 16
        ang = (pos * inv_freq[fi]).astype(np.float32)
        C[d, CT:] = np.cos(ang)
        S[d, CT:] = np.sin(ang) * (-1.0 if (d % 32) < 16 else 1.0)
    ropeC = np.ascontiguousarray(np.concatenate([C, C], 0)); ropeS = np.ascontiguousarray(np.concatenate([S, S], 0))
    idx = np.arange(128)
    gmask = np.zeros((128, 7, 128), np.float32)
    md = lambda sz: (idx[:, None] // sz == idx[None, :] // sz).astype(np.float32)
    gmask[:, 0, :] = md(2)
    for k_, sz in enumerate((2, 4, 8, 16, 32, 64)):
        gmask[:, 1 + k_, :] = md(2 * sz) - md(sz)
    shared = dict(gmask=gmask, w_mod=f(inp["w_mod"]), vecs=vecs, gdn_g=f(inp["gdn_norm"]), gatec=np.ascontiguousarray(gatec), w_in=w_in_ext,
                  w_out=f(inp["w_out"]), ffn_g=f(inp["ffn_w_gate"]), ffn_u=f(inp["ffn_w_up"]), ffn_d=f(inp["ffn_w_down"]),
                  router=f(inp["router"]), moe_g=f(inp["moe_w_gate"]), moe_u=f(inp["moe_w_up"]), moe_d=f(inp["moe_w_down"]),
                  ropeC=ropeC, ropeS=ropeS)
    x, ctx, cc, c_ctx = f(inp["x"]), f(inp["ctx"]), f(inp["c"]), f(inp["c_ctx"])
    maps = []
    for core in range(8):
        b0 = 2 * core
        cvec = np.zeros((4, D), np.float32)
        cvec[0], cvec[1], cvec[2] = cc[b0], cc[b0 + 1], c_ctx
        d = dict(shared)
        d.update(x=np.ascontiguousarray(x[b0:b0 + 2]), ctx=np.ascontiguousarray(ctx[b0:b0 + 2]), cvec=cvec)
        maps.append(d)
    return maps


_CACHE = {}


def kernel(**inputs):
    maps = host_prep(inputs)
    if "m" not in _CACHE:
        _CACHE["m"] = build()
    m = _CACHE["m"]
    res = run_bass_kernel_spmd(m.nc, maps, core_ids=list(range(8)))
    return np.concatenate([r["out"] for r in res.results], axis=0)


def norm_mod(m, l, which, xt, sq, hT, pss, Tt, gm, sh_j0, hf=None):
    P, c = m.P, m.c
    P.add("act", lambda e: e.activation(out=sq[:, :, 0:Tt], in_=xt[:, :, 0:Tt], func=AF.Square), [xt], [sq])
    for kc in range(8):
        P.add("pe", (lambda kc: (lambda e: e.matmul(pss[:, 0:Tt], lhsT=c.onesb[:, :], rhs=sq[:, kc, 0:Tt], start=(kc == 0), stop=(kc == 7))))(kc), [sq, c.onesb], [pss])
    rs = m.rs
    P.add("act", lambda e: e.activation(out=rs[:, 0:Tt], in_=pss[:, 0:Tt], func=AF.Sqrt, scale=1.0 / D, bias=m.epsc[:, 0:1]), [pss, m.epsc], [rs])
    P.add("dve", lambda e: e.reciprocal(out=rs[:, 0:Tt], in_=rs[:, 0:Tt]), [rs], [rs])
    tmp = m.nm_tmp
    for kc in range(8):
        P.add("dve", (lambda kc: (lambda e: e.scalar_tensor_tensor(out=tmp[kc % 2][:, 0:Tt], in0=xt[:, kc, 0:Tt], scalar=gm[:, l, kc, which:which + 1], in1=rs[:, 0:Tt],
                                                                   op0=ALU.mult, op1=ALU.mult)))(kc), [xt, rs], [tmp[kc % 2]])
        dst = hT if hf is None else hf
        P.add("act", (lambda kc, dst: (lambda e: e.activation(out=dst[:, kc, 0:Tt], in_=tmp[kc % 2][:, 0:Tt], func=AF.Identity,
                                                              bias=c.modv[:, l, sh_j0 + kc, which:which + 1], scale=1.0)))(kc, dst), [tmp[kc % 2], c.modv], [dst])
    if hf is not None:
        P.add("pool", lambda e: e.tensor_copy(out=hT[:, :, 0:Tt], in_=hf[:, :, 0:Tt]), [hf], [hT])


def alloc_norm(m):
    P = m.P
    m.rs = P.sb("rs", [128, 512], F32)
    m.nm_tmp = [P.sb(f"nmt{i}", [128, 512], F32) for i in range(2)]
    m.epsc = P.sb("epsc", [128, 1], F32)
    P.add("pool", lambda e: e.memset(m.epsc[:, :], EPS), [], [m.epsc])


def stage_proj(m, l):
    P, c = m.P, m.c
    if not getattr(m, "nocast", False):
        if IMPLEMENTED_FFN:
            casts_ffn(m, l)
        if l + 1 < NL:
            casts_proj(m, l + 1)
    alloc_norm(m)
    st_v = P.sb("st_v", [128, 4, 128], BF16)
    W = P.sb("Win", [128, 8, NCOLS], BF16)
    for kc in range(8):
        P.dma("sp", W[:, kc, :], m.w_in_b[l][kc * 128:(kc + 1) * 128, :], [P.R(("w_in_b", l))], [W], "Win")
    xts = [P.sb(f"xt{i}", [128, 8, 512], F32) for i in range(2)]
    tabs = [P.sb(f"tab{i}", [128, 2, 512], F32) for i in range(2)]
    sq = P.sb("sq", [128, 8, 512], BF16)
    hT = P.sb("hT", [128, 8, 512], BF16)
    st_qkv = P.sb("st_qkv", [128, 6, 512], F32)
    st_q = P.sb("st_q", [128, 4, 512], BF16)
    st_k = P.sb("st_k", [128, 2, 512], BF16)
    st_cb = P.sb("st_cb", [128, 2, 512], F32)
    st_pp = P.sb("st_pp", [128, 2, 512], F32)
    st_z = P.sb("st_z", [128, 4, 256], F32)
    st_g = P.sb("st_g", [8, 2, 512], F32)
    zT = P.sb("zT", [128, 3, 512], F32)
    gt = [P.sb(f"gt{i}", [8, 512], F32) for i in range(3)]
    sqq = P.sb("sqq", [128, 512], BF16)
    rq = P.sb("rq", [128, 512], F32)
    t1 = P.sb("t1", [128, 512], F32)
    t2 = P.sb("t2", [128, 512], F32)
    ccs = P.sb("ccs", [128, 512], F32)
    ps = [P.ps(f"ps{i}", [128, 512]) for i in range(8)]
    it = 0
    tl = [(b, t0, Tt) for b in range(2) for (t0, Tt) in tiles_of_batch()]

    def load(i):
        b, t0, Tt = tl[i]
        P.dma("sp", xts[i % 2][:, :, 0:Tt], m.XT[b].rearrange("(c p) t -> p c t", p=128)[:, :, t0:t0 + Tt], [P.R(("XT", b, t0))], [xts[i % 2]], ("ldx", i % 2))
        P.dma("sp", tabs[i % 2][:, 0, 0:Tt], m.ropeC[:, t0:t0 + Tt], [], [tabs[i % 2]], ("ldt", i % 2))
        P.dma("sp", tabs[i % 2][:, 1, 0:Tt], m.ropeS[:, t0:t0 + Tt], [], [tabs[i % 2]], ("ldt", i % 2))

    def body(i, b, t0, Tt):
        xt, tab = xts[i % 2], tabs[i % 2]
        which = 2 if t0 < CT else b
        norm_mod(m, l, which, xt, sq, hT, ps[7], Tt, c.gm1, 0)
        nsub = Tt // 128

        def fm(j, pst):
            for kc in range(8):
                P.add("pe", (lambda kc: (lambda e: e.matmul(pst[:, 0:Tt], lhsT=W[:, kc, j * 128:(j + 1) * 128], rhs=hT[:, kc, 0:Tt], start=(kc == 0), stop=(kc == 7))))(kc), [W, hT], [pst])

        for j in range(6):
            pst = ps[j % 2]
            fm(j, pst)
            if j % 2 == 0:
                P.add("dve", (lambda j, pst: (lambda e: e.tensor_copy(out=st_qkv[:, j, 0:Tt], in_=pst[:, 0:Tt])))(j, pst), [pst], [st_qkv])
            else:
                P.add("act", (lambda j, pst: (lambda e: e.activation(out=st_qkv[:, j, 0:Tt], in_=pst[:, 0:Tt], func=AF.Copy)))(j, pst), [pst], [st_qkv])
        P.dma("sp", m.QKVp[b].rearrange("(c p) t -> p c t", p=128)[:, :, pc(t0):pc(t0) + Tt], st_qkv[:, :, 0:Tt], [st_qkv], [P.R(("QKVp", b, t0))], "st_qkv")

        if SEC < 2: return
        qkc = [0]

        def qk_chunk(jq, jp, rg, rgp, dst, stb):
            pq, pp_ = (ps[2], ps[3]) if qkc[0] % 2 == 0 else (ps[5], ps[6])
            qkc[0] += 1
            fm(jq, pq)
            fm(jp, pp_)
            P.add("act", lambda e: e.activation(out=sqq[:, 0:Tt], in_=pq[:, 0:Tt], func=AF.Square), [pq], [sqq])
            P.add("pe", lambda e: e.matmul(ps[4][:, 0:Tt], lhsT=c.bdiag[:, :], rhs=sqq[:, 0:Tt], start=True, stop=True), [sqq, c.bdiag], [ps[4]])
            P.add("act", lambda e: e.activation(out=rq[:, 0:Tt], in_=ps[4][:, 0:Tt], func=AF.Sqrt, scale=1.0 / 64, bias=m.epsc[:, 0:1]), [ps[4], m.epsc], [rq])
            P.add("dve", lambda e: e.reciprocal(out=rq[:, 0:Tt], in_=rq[:, 0:Tt]), [rq], [rq])
            P.add("dve", lambda e: e.scalar_tensor_tensor(out=t1[:, 0:Tt], in0=pq[:, 0:Tt], scalar=c.vcol[:, l, rg:rg + 1], in1=tab[:, 0, 0:Tt], op0=ALU.mult, op1=ALU.mult), [pq, tab, c.vcol], [t1])
            P.add("dve", lambda e: e.scalar_tensor_tensor(out=t2[:, 0:Tt], in0=pp_[:, 0:Tt], scalar=c.vcol[:, l, rgp:rgp + 1], in1=tab[:, 1, 0:Tt], op0=ALU.mult, op1=ALU.mult), [pp_, tab, c.vcol], [t2])
            P.add("pool", lambda e: e.tensor_tensor(out=t1[:, 0:Tt], in0=t1[:, 0:Tt], in1=t2[:, 0:Tt], op=ALU.add), [t1, t2], [t1])
            P.add("pool", lambda e: e.tensor_tensor(out=dst, in0=t1[:, 0:Tt], in1=rq[:, 0:Tt], op=ALU.mult), [t1, rq], [stb])

        for jq in range(4):
            qk_chunk(6 + jq, 10 + jq, R_QG, R_QGP, st_q[:, jq, 0:Tt], st_q)
        P.dma("sp", m.QA[b].rearrange("(c p) t -> p c t", p=128)[:, :, t0:t0 + Tt], st_q[:, :, 0:Tt], [st_q], [P.R(("QA", b, t0))], "st_q")
        for jk in range(2):
            qk_chunk(14 + jk, 16 + jk, R_KG, R_KGP, st_k[:, jk, 0:Tt], st_k)
        P.dma("sp", m.KA[b].rearrange("s p t -> p s t")[:, :, t0:t0 + Tt], st_k[:, :, 0:Tt], [st_k], [P.R(("KA", b, t0))], "st_k")

        if SEC < 3: return
        for j in range(2):
            fm(18 + j, ps[j % 2])
            P.add("act", (lambda j: (lambda e: e.activation(out=st_cb[:, j, 0:Tt], in_=ps[j % 2][:, 0:Tt], func=AF.Copy)))(j), [ps[j % 2]], [st_cb])
        P.dma("sp", m.CB[b].rearrange("(c p) t -> p c t", p=128)[:, :, t0:t0 + Tt], st_cb[:, :, 0:Tt], [st_cb], [P.R(("CB", b, t0))], "st_cb")
        for j in range(2):
            fm(20 + j, ps[2])
            fm(22 + j, ps[3])
            P.add("act", lambda e: e.activation(out=ccs[:, 0:Tt], in_=ps[2][:, 0:Tt], func=AF.Copy), [ps[2]], [ccs])
            P.add("dve", (lambda j: (lambda e: e.tensor_tensor(out=st_pp[:, j, 0:Tt], in0=ps[3][:, 0:Tt], in1=ccs[:, 0:Tt], op=ALU.mult)))(j), [ps[3], ccs], [st_pp])
        P.dma("sp", m.PP[b].rearrange("(c p) t -> p c t", p=128)[:, :, pc(t0):pc(t0) + Tt], st_pp[:, :, 0:Tt], [st_pp], [P.R(("PP", b, t0))], "st_pp")

        if SEC < 4: return
        for j in range(3):
            pst = ps[j % 2]
            c0 = NFM * 128 + (j * 128 if j < 2 else 272)
            c0 = c0 - C0SHIFT
            for kc in range(8):
                P.add("pe", (lambda kc, c0, pst: (lambda e: e.matmul(pst[:, 0:Tt], lhsT=W[:, kc, c0:c0 + 128], rhs=hT[:, kc, 0:Tt], start=(kc == 0), stop=(kc == 7))))(kc, c0, pst), [W, hT], [pst])
            if j < 2:
                P.add("act", (lambda j, pst: (lambda e: e.activation(out=zT[:, j, 0:Tt], in_=pst[:, 0:Tt], func=AF.Sigmoid)))(j, pst), [pst], [zT])
                P.add("dve", (lambda j, pst: (lambda e: e.tensor_tensor(out=zT[:, j, 0:Tt], in0=pst[:, 0:Tt], in1=zT[:, j, 0:Tt], op=ALU.mult)))(j, pst), [pst, zT], [zT])
            else:
                P.add("act", (lambda j, pst: (lambda e: e.activation(out=zT[:, j, 0:Tt], in_=pst[:, 0:Tt], func=AF.Copy)))(j, pst), [pst], [zT])
        if SUB < 1: return
        for n in range(nsub):
            pst = ps[2 + (n % 2)]
            pv = ps[5 + (n % 2)]
            for j in range(2):
                P.add("pe", (lambda n, j, pst: (lambda e: e.transpose(out=pst[:, j * 128:(j + 1) * 128], in_=zT[:, j, n * 128:(n + 1) * 128], identity=c.ident[:, :])))(n, j, pst), [zT, c.ident], [pst])
            P.add("pe", (lambda n, pv: (lambda e: e.transpose(out=pv[:, 0:128], in_=zT[:, 2, n * 128:(n + 1) * 128], identity=c.ident[:, :])))(n, pv), [zT, c.ident], [pv])
            P.add("dve", (lambda n, pst: (lambda e: e.tensor_copy(out=st_z[:, n, :], in_=pst[:, 0:256])))(n, pst), [pst], [st_z])
            P.add("act", (lambda n, pv: (lambda e: e.activation(out=st_v[:, n, :], in_=pv[:, 0:128], func=AF.Copy)))(n, pv), [pv], [st_v])
        if SUB < 3: return
        P.dma("sp", m.Zs[b, t0:t0 + Tt, :].rearrange("(n p) f -> p n f", p=128), st_z[:, 0:nsub, :], [st_z], [P.R(("Zs", b, t0))], "st_z")
        P.dma("sp", m.VA[b, t0:t0 + Tt, :].rearrange("(n p) f -> p n f", p=128), st_v[:, 0:nsub, :], [st_v], [P.R(("VA", b, t0))], "st_v")
        if SEC < 5: return
        for j in range(2):
            c0 = NFM * 128 + 256 + j * 8
            for kc in range(8):
                P.add("pe", (lambda kc, c0, j: (lambda e: e.matmul(ps[2 + j][0:8, 0:Tt], lhsT=W[:, kc, c0:c0 + 8], rhs=hT[:, kc, 0:Tt], start=(kc == 0), stop=(kc == 7))))(kc, c0, j), [W, hT], [ps[2 + j]])
        g0, g1, g2 = gt
        P.add("act", lambda e: e.activation(out=g0[0:8, 0:Tt], in_=ps[2][0:8, 0:Tt], func=AF.Identity, bias=m.gcol[0:8, l, 0:1], scale=1.0), [ps[2], m.gcol], [g0])
        P.add("act", lambda e: e.activation(out=g1[0:8, 0:Tt], in_=g0[0:8, 0:Tt], func=AF.Abs), [g0], [g1])
        P.add("act", lambda e: e.activation(out=g1[0:8, 0:Tt], in_=g1[0:8, 0:Tt], func=AF.Exp, scale=-1.0), [g1], [g1])
        P.add("act", lambda e: e.activation(out=g1[0:8, 0:Tt], in_=g1[0:8, 0:Tt], func=AF.Ln, bias=1.0), [g1], [g1])
        P.add("dve", lambda e: e.scalar_tensor_tensor(out=g2[0:8, 0:Tt], in0=g0[0:8, 0:Tt], scalar=0.0, in1=g1[0:8, 0:Tt], op0=ALU.max, op1=ALU.add), [g0, g1], [g2])
        P.add("dve", lambda e: e.tensor_scalar(out=st_g[0:8, 0, 0:Tt], in0=g2[0:8, 0:Tt], scalar1=m.gcol[0:8, l, 1:2], scalar2=None, op0=ALU.mult), [g2, m.gcol], [st_g])
        P.add("act", lambda e: e.activation(out=st_g[0:8, 1, 0:Tt], in_=ps[3][0:8, 0:Tt], func=AF.Sigmoid), [ps[3]], [st_g])
        P.dma("sp", m.G[b].rearrange("(j p) t -> p j t", p=8)[:, :, t0:t0 + Tt], st_g[0:8, :, 0:Tt], [st_g], [P.R(("G", b, t0))], "st_g")

    tl = tl[:NTILES]
    load(0)
    for i, (b, t0, Tt) in enumerate(tl):
        if i + 1 < len(tl):
            load(i + 1)
        body(i, b, t0, Tt)


def stage_attn(m, l, last):
    P, c = m.P, m.c
    NKC = T // 128
    kT = P.sb("kT", [128, 2, T], BF16)
    vS = P.sb("vS", [128, NKC, 2, 65], BF16)
    qTs = [[P.sb(f"qT{i}_{h}", [128, 512], BF16) for h in range(2)] for i in range(2)]
    for i_ in range(2):
        for h_ in range(2):
            P.add("pool", (lambda i_, h_: (lambda e: e.memset(qTs[i_][h_][:, :], 0.0)))(i_, h_), [], [qTs[i_][h_]])
    pTs = [P.sb(f"pT{i}", [128, 512], BF16) for i in range(5)]
    rdn = P.sb("rdn", [128, 512], F32)
    oS = P.sb("oS", [64, 512], F32)
    ys = [P.sb(f"ys{i}", [64, 512], BF16) for i in range(2)]
    ps_s = [P.ps(f"ps_s{i}", [128, 512]) for i in range(5)]
    ps_o = [P.ps(f"ps_o{i}", [128, 512]) for i in range(2)]
    ps_b = P.ps("ps_b", [128, 512])
    cnt = [0, 0]
    for b in range(2):
        P.add("pool", lambda e: e.memset(vS[:, :, :, 64:65], 1.0), [], [vS])
        for s_ in range(2):
            P.dma("sp", kT[:, s_, :], m.KA[b, s_], [P.R(("KA", b, t0)) for (t0, _) in tiles_of_batch()], [kT], "kT")
        for h_ in range(2):
            P.dma("sp", vS[:, :, h_, 0:64], m.VA[b][:, h_ * 64:(h_ + 1) * 64].rearrange("(n p) d -> p n d", p=128), [P.R(("VA", b, t0)) for (t0, _) in tiles_of_batch()], [vS], "vS")
        work = [(qc, t0, Tt) for qc in range(4) for (t0, Tt) in tiles_of_batch() if not (last and t0 < CT)]

        def loadq(i):
            qc, t0, Tt = work[i]
            for h_ in range(2):
                P.dma("sp", qTs[i % 2][h_][h_ * 64:(h_ + 1) * 64, 0:Tt], m.QA[b][qc * 128 + h_ * 64:qc * 128 + (h_ + 1) * 64, t0:t0 + Tt], [P.R(("QA", b, t0))], [qTs[i % 2][h_]], ("ldq", i % 2, h_))

        def body(i, qc, t0, Tt):
            kv = qc // 2
            kcs = list(range(2)) if t0 < CT else list(range(NKC))
            def head(hh):
                idx = 0 if kv == hh else 1
                qT = qTs[i % 2][hh]
                po = ps_o[hh]
                r0 = hh * 64
                NB_ = 5
                base_ = cnt[0]
                cnt[0] += len(kcs)

                def s_stage(n_):
                    kc = kcs[n_]
                    pss = ps_s[(base_ + n_) % NB_]
                    pT = pTs[(base_ + n_) % NB_]
                    P.add("pe", lambda e: e.matmul(pss[:, 0:Tt], lhsT=kT[:, idx, kc * 128:(kc + 1) * 128], rhs=qT[:, 0:Tt], start=True, stop=True), [kT, qT], [pss])
                    P.add("act", lambda e: e.activation(out=pT[:, 0:Tt], in_=pss[:, 0:Tt], func=AF.Exp, scale=0.125), [pss], [pT])

                def pv_stage(n_):
                    kc = kcs[n_]
                    pT = pTs[(base_ + n_) % NB_]
                    P.add("pe", lambda e: e.matmul(po[0:65, 0:Tt], lhsT=vS[:, kc, kv, 0:65], rhs=pT[:, 0:Tt], start=(n_ == 0), stop=(n_ == len(kcs) - 1)), [vS, pT], [po])

                LA_ = NB_ - 1
                for n_ in range(min(LA_, len(kcs))):
                    s_stage(n_)
                for n_ in range(len(kcs)):
                    if n_ + LA_ < len(kcs):
                        s_stage(n_ + LA_)
                    pv_stage(n_)
                y = ys[cnt[1] % 2]
                cnt[1] += 1
                P.add("dve", lambda e: e.reciprocal(out=rdn[64:65, 0:Tt], in_=po[64:65, 0:Tt]), [po], [rdn])
                P.add("pe", lambda e: e.matmul(ps_b[0:64, 0:Tt], lhsT=c.onesf[64:65, 0:64], rhs=rdn[64:65, 0:Tt], start=True, stop=True), [rdn, c.onesf], [ps_b])
                P.add("act", lambda e: e.activation(out=oS[:, 0:Tt], in_=po[0:64, 0:Tt], func=AF.Copy), [po], [oS])
                P.add("dve", (lambda y: (lambda e: e.tensor_tensor(out=y[:, 0:Tt], in0=ps_b[0:64, 0:Tt], in1=oS[:, 0:Tt], op=ALU.mult)))(y), [ps_b, oS], [y])
                hrow = 256 + (2 * qc + hh) * 64
                P.dma("sp", m.YT[b][hrow:hrow + 64, t0:t0 + Tt], y[:, 0:Tt], [y], [P.R(("YT", b, t0, hrow))], ("sty", cnt[1] % 2))

            head(0)
            head(1)

        loadq(0)
        for i, (qc, t0, Tt) in enumerate(work):
            if i + 1 < len(work):
                loadq(i + 1)
            body(i, qc, t0, Tt)


def epilogue(m):
    P, c = m.P, m.c
    alloc_norm(m)
    xts = [P.sb(f"ext{i}", [128, 8, 512], F32) for i in range(2)]
    sq = P.sb("esq", [128, 8, 512], BF16)
    yT = P.sb("eyT", [128, 8, 512], F32)
    ost = [P.sb(f"eost{i}", [128, D], F32) for i in range(2)]
    ps = [P.ps(f"eps{i}", [128, 512]) for i in range(8)]
    tl = [(b, t0, Tt) for b in range(2) for (t0, Tt) in tiles_of_batch() if t0 >= CT]
    cnt = [0]

    def load(i):
        b, t0, Tt = tl[i]
        P.dma("sp", xts[i % 2][:, :, 0:Tt], m.XT[b].rearrange("(c p) t -> p c t", p=128)[:, :, t0:t0 + Tt], [P.R(("XT", b, t0))], [xts[i % 2]], ("ldx", i % 2))

    def body(i, b, t0, Tt):
        xt = xts[i % 2]
        pss = ps[7]
        P.add("act", lambda e: e.activation(out=sq[:, :, 0:Tt], in_=xt[:, :, 0:Tt], func=AF.Square), [xt], [sq])
        for kc in range(8):
            P.add("pe", (lambda kc: (lambda e: e.matmul(pss[:, 0:Tt], lhsT=c.onesb[:, :], rhs=sq[:, kc, 0:Tt], start=(kc == 0), stop=(kc == 7))))(kc), [sq, c.onesb], [pss])
        rs = m.rs
        P.add("act", lambda e: e.activation(out=rs[:, 0:Tt], in_=pss[:, 0:Tt], func=AF.Sqrt, scale=1.0 / D, bias=m.epsc[:, 0:1]), [pss, m.epsc], [rs])
        P.add("dve", lambda e: e.reciprocal(out=rs[:, 0:Tt], in_=rs[:, 0:Tt]), [rs], [rs])
        for kc in range(8):
            P.add("dve", (lambda kc: (lambda e: e.scalar_tensor_tensor(out=yT[:, kc, 0:Tt], in0=xt[:, kc, 0:Tt], scalar=c.vcol[:, 0, R_NF + kc:R_NF + kc + 1], in1=rs[:, 0:Tt],
                                                                       op0=ALU.mult, op1=ALU.mult)))(kc), [xt, rs, c.vcol], [yT])
        for n in range(Tt // 128):
            o = ost[cnt[0] % 2]
            cnt[0] += 1
            for half in range(2):
                pt = ps[2 * (n % 2) + half]
                for j in range(4):
                    kc = half * 4 + j
                    P.add("pe", (lambda kc, j, pt, n: (lambda e: e.transpose(out=pt[:, j * 128:(j + 1) * 128], in_=yT[:, kc, n * 128:(n + 1) * 128], identity=c.ident[:, :])))(kc, j, pt, n), [yT, c.ident], [pt])
                if half == 0:
                    P.add("dve", (lambda pt, o: (lambda e: e.tensor_copy(out=o[:, 0:512], in_=pt[:, 0:512])))(pt, o), [pt], [o])
                else:
                    P.add("act", (lambda pt, o: (lambda e: e.activation(out=o[:, 512:1024], in_=pt[:, 0:512], func=AF.Copy)))(pt, o), [pt], [o])
            tt = t0 - CT + n * 128
            P.dma("sp", m.out[b, tt:tt + 128, :], o[:, :], [o], [P.R(("out", b, tt))], ("sto", cnt[0] % 2))

    load(0)
    for i, (b, t0, Tt) in enumerate(tl):
        if i + 1 < len(tl):
            load(i + 1)
        body(i, b, t0, Tt)


def ffn_blocks(m, l):
    i = l // 2
    blocks = []
    if l % 2 == 0:
        for (f0, nf) in ((0, 6), (768, 5), (1408, 6), (2176, 5)):
            w = nf * 128
            blocks.append((m.ffn_g_b[i][:, f0:f0 + w], m.ffn_u_b[i][:, f0:f0 + w], m.ffn_d_b[i][f0:f0 + w, :], nf, None,
                           [("ffn_g_b", i), ("ffn_u_b", i), ("ffn_d_b", i)]))
    else:
        for e in range(NE):
            for (f0, nf) in ((0, 6), (768, 5)):
                w = nf * 128
                blocks.append((m.moe_g_b[i, e][:, f0:f0 + w], m.moe_u_b[i, e][:, f0:f0 + w], m.moe_d_b[i, e][f0:f0 + w, :], nf, e,
                               [("moe_g_b", i, e), ("moe_u_b", i, e), ("moe_d_b", i, e)]))
    return blocks


def stage_ffn(m, l, last):
    P, c = m.P, m.c
    moe = (l % 2 == 1)
    alloc_norm(m)
    Wout = P.sb("Wout", [128, 8, D], BF16)
    P.dma("sp", Wout[:, :, :], m.w_out_b[l].rearrange("(k p) n -> p k n", p=128), [P.R(("w_out_b", l))], [Wout], "Wout")
    wg = [P.sb(f"wg{i}", [128, 8, 768], BF16) for i in range(2)]
    wu = [P.sb(f"wu{i}", [128, 8, 768], BF16) for i in range(2)]
    wd = [P.sb(f"wd{i}", [128, 6, D], BF16) for i in range(2)]
    xt = P.sb("fxt", [128, 8, 512], F32)
    yT = P.sb("fyT", [128, 8, 512], BF16)
    cb = P.sb("fcb", [128, 2, 512], F32)
    pp = P.sb("fpp", [128, 2, 514], F32)
    cacc = P.sb("fcacc", [128, 512], F32)
    sq = P.sb("fsq", [128, 8, 512], BF16)
    acc = P.sb("facc", [128, 8, 512], F32)
    h2b = P.sb("fh2b", [128, 8, 512], BF16)
    act = P.sb("fact", [128, 6, 512], BF16)
    sg = [P.sb(f"fsg{i}", [128, 512], F32) for i in range(2)]
    a1 = P.sb("fa1", [128, 512], F32)
    cB = P.sb("fcB", [128, 512], F32)
    combT = P.sb("fcombT", [8, 512], F32)
    combs = P.sb("fcombs", [128, 4, 8], F32)
    rt = [P.sb(f"frt{i}", [128, 8], F32) for i in range(4)]
    rsc = [P.sb(f"frsc{i}", [128, 1], F32) for i in range(3)]
    if moe:
        Rw = P.sb("fRw", [128, 8, NE], F32)
        P.dma("sp", Rw[:, :, :], m.router[l // 2].rearrange("(k p) n -> p k n", p=128), [], [Rw], "Rw")
    pg = [P.ps(f"fpg{i}", [128, 512]) for i in range(2)]
    pu = [P.ps(f"fpu{i}", [128, 512]) for i in range(2)]
    pd = [P.ps(f"fpd{i}", [128, 512]) for i in range(2)]
    pm = [P.ps(f"fpm{i}", [128, 512]) for i in range(2)]
    blocks = ffn_blocks(m, l)
    groups = [(b, t0, Tt) for b in range(2) for (t0, Tt) in tiles_of_batch() if not (last and t0 < CT)]
    seq = [(gi, bi) for gi in range(len(groups)) for bi in range(len(blocks))]
    cnt = [0]

    def loadw(si):
        gi, bi = seq[si]
        gs, us, ds, nf, e, keys = blocks[bi]
        k = si % 2
        rd = [P.R(kk) for kk in keys]
        P.dma("sp", wg[k][:, :, 0:nf * 128], gs.rearrange("(k p) n -> p k n", p=128), rd, [wg[k]], ("wg", k))
        P.dma("sp", wu[k][:, :, 0:nf * 128], us.rearrange("(k p) n -> p k n", p=128), rd, [wu[k]], ("wu", k))
        P.dma("sp", wd[k][:, 0:nf, :], ds.rearrange("(f p) n -> p f n", p=128), rd, [wd[k]], ("wd", k))

    def load_aux(gj):
        b_, t0_, Tt_ = groups[gj]
        P.dma("sp", yT[:, 0:6, 0:Tt_], m.YT[b_][0:768, :].rearrange("(c p) t -> p c t", p=128)[:, :, t0_:t0_ + Tt_], [P.R(("YT", b_, t0_))], [yT], "fyT")
        P.dma("sp", cb[:, :, 0:Tt_], m.CB[b_].rearrange("(c p) t -> p c t", p=128)[:, :, t0_:t0_ + Tt_], [P.R(("CB", b_, t0_))], [cb], "fcb")
        P.dma("sp", pp[:, :, 0:Tt_ + 2], m.PP[b_].rearrange("(c p) t -> p c t", p=128)[:, :, pc(t0_) - 1:pc(t0_) + Tt_ + 1], [P.R(("PP", b_, t0_))], [pp], "fpp")

    def group(gi, b, t0, Tt):
        which = 2 if t0 < CT else b
        XTv = m.XT[b].rearrange("(c p) t -> p c t", p=128)
        P.dma("sp", xt[:, :, 0:Tt], XTv[:, :, t0:t0 + Tt], [P.R(("XT", b, t0))], [xt], "fxt")
        if gi == 0:
            load_aux(0)
        for j in range(2):
            w = lambda k, j=j: c.vcol[:, l, R_CC + k * 2 + j:R_CC + k * 2 + j + 1]
            P.add("dve", (lambda j: (lambda e: e.tensor_scalar(out=cacc[:, 0:Tt], in0=pp[:, j, 0:Tt], scalar1=w(0, j), scalar2=None, op0=ALU.mult)))(j), [pp, c.vcol], [cacc])
            P.add("dve", (lambda j: (lambda e: e.scalar_tensor_tensor(out=cacc[:, 0:Tt], in0=pp[:, j, 1:Tt + 1], scalar=w(1, j), in1=cacc[:, 0:Tt], op0=ALU.mult, op1=ALU.add)))(j), [pp, cacc, c.vcol], [cacc])
            P.add("dve", (lambda j: (lambda e: e.scalar_tensor_tensor(out=cacc[:, 0:Tt], in0=pp[:, j, 2:Tt + 2], scalar=w(2, j), in1=cacc[:, 0:Tt], op0=ALU.mult, op1=ALU.add)))(j), [pp, cacc, c.vcol], [cacc])
            P.add("dve", (lambda j: (lambda e: e.tensor_tensor(out=yT[:, 6 + j, 0:Tt], in0=cacc[:, 0:Tt], in1=cb[:, j, 0:Tt], op=ALU.mult)))(j), [cacc, cb], [yT])
        for oc in range(8):
            ps_ = pm[oc % 2]
            for kc in range(8):
                P.add("pe", (lambda oc, kc, ps_: (lambda e: e.matmul(ps_[:, 0:Tt], lhsT=Wout[:, kc, oc * 128:(oc + 1) * 128], rhs=yT[:, kc, 0:Tt], start=(kc == 0), stop=(kc == 7))))(oc, kc, ps_), [Wout, yT], [ps_])
            P.add("dve", (lambda oc, ps_: (lambda e: e.scalar_tensor_tensor(out=xt[:, oc, 0:Tt], in0=ps_[:, 0:Tt], scalar=c.modv[:, l, 16 + oc, which:which + 1], in1=xt[:, oc, 0:Tt],
                                                                            op0=ALU.mult, op1=ALU.add)))(oc, ps_), [ps_, xt, c.modv], [xt])
        norm_mod(m, l, which, xt, sq, h2b, pm[0], Tt, c.gm2, 24, hf=(acc if moe else None))
        if moe:
            nsub = Tt // 128

            def route(n):
                pr = pm[1]
                for kc in range(8):
                    P.add("pe", (lambda kc: (lambda e: e.matmul(pr[:, 0:NE], lhsT=acc[:, kc, n * 128:(n + 1) * 128], rhs=Rw[:, kc, :], start=(kc == 0), stop=(kc == 7))))(kc), [acc, Rw], [pr])
                mx, ex, mk, em = rt
                nm1, ss, ri = rsc
                P.add("dve", lambda e: e.max(out=mx[:, :], in_=pr[:, 0:NE]), [pr], [mx])
                P.add("dve", lambda e: e.tensor_scalar(out=nm1[:, :], in0=mx[:, 0:1], scalar1=-1.0, scalar2=None, op0=ALU.mult), [mx], [nm1])
                P.add("act", lambda e: e.activation(out=ex[:, :], in_=pr[:, 0:NE], func=AF.Exp, bias=nm1[:, 0:1], scale=1.0), [pr, nm1], [ex])
                P.add("dve", lambda e: e.tensor_scalar(out=mk[:, :], in0=pr[:, 0:NE], scalar1=mx[:, 1:2], scalar2=None, op0=ALU.is_ge), [pr, mx], [mk])
                P.add("dve", lambda e: e.tensor_tensor(out=em[:, :], in0=ex[:, :], in1=mk[:, :], op=ALU.mult), [ex, mk], [em])
                P.add("dve", lambda e: e.reduce_sum(out=ss[:, :], in_=em[:, :], axis=AX.X), [em], [ss])
                P.add("dve", lambda e: e.reciprocal(out=ri[:, :], in_=ss[:, :]), [ss], [ri])
                P.add("dve", lambda e: e.tensor_scalar(out=combs[:, n, :], in0=em[:, :], scalar1=ri[:, 0:1], scalar2=None, op0=ALU.mult), [em, ri], [combs])

            for n in range(nsub):
                route(n)
            pt = pm[1]
            for n in range(nsub):
                P.add("pe", (lambda n: (lambda e: e.transpose(out=pt[0:NE, n * 128:(n + 1) * 128], in_=combs[:, n, :], identity=c.ident[:, :])))(n), [combs, c.ident], [pt])
            P.add("dve", lambda e: e.tensor_copy(out=combT[:, 0:Tt], in_=pt[0:NE, 0:Tt]), [pt], [combT])

        def block(bi):
            si = cnt[0]
            cnt[0] += 1
            if si + 1 < len(seq):
                loadw(si + 1)
            gs, us, ds, nf, ex_, keys = blocks[bi]
            k = si % 2
            G_, U_, D_ = wg[k], wu[k], wd[k]
            if ex_ is not None and (bi % 2 == 0):
                pcb = pm[1]
                P.add("pe", lambda e: e.matmul(pcb[:, 0:Tt], lhsT=c.sel[0:NE, ex_, :], rhs=combT[0:NE, 0:Tt], start=True, stop=True), [combT, c.sel], [pcb])
                P.add("act", lambda e: e.activation(out=cB[:, 0:Tt], in_=pcb[:, 0:Tt], func=AF.Copy), [pcb], [cB])

            def fchunk(fc):
                g_, u_, s_ = pg[fc % 2], pu[fc % 2], sg[fc % 2]
                for kc in range(8):
                    P.add("pe", (lambda kc: (lambda e: e.matmul(g_[:, 0:Tt], lhsT=G_[:, kc, fc * 128:(fc + 1) * 128], rhs=h2b[:, kc, 0:Tt], start=(kc == 0), stop=(kc == 7))))(kc), [G_, h2b], [g_])
                for kc in range(8):
                    P.add("pe", (lambda kc: (lambda e: e.matmul(u_[:, 0:Tt], lhsT=U_[:, kc, fc * 128:(fc + 1) * 128], rhs=h2b[:, kc, 0:Tt], start=(kc == 0), stop=(kc == 7))))(kc), [U_, h2b], [u_])
                P.add("act", lambda e: e.activation(out=s_[:, 0:Tt], in_=g_[:, 0:Tt], func=AF.Silu), [g_], [s_])
                if ex_ is None:
                    P.add("dve", lambda e: e.tensor_tensor(out=act[:, fc, 0:Tt], in0=u_[:, 0:Tt], in1=s_[:, 0:Tt], op=ALU.mult), [u_, s_], [act])
                else:
                    P.add("dve", lambda e: e.tensor_tensor(out=a1[:, 0:Tt], in0=u_[:, 0:Tt], in1=s_[:, 0:Tt], op=ALU.mult), [u_, s_], [a1])
                    P.add("pool", lambda e: e.tensor_tensor(out=act[:, fc, 0:Tt], in0=a1[:, 0:Tt], in1=cB[:, 0:Tt], op=ALU.mult), [a1, cB], [act])

            for fc in range(nf):
                fchunk(fc)

            def dchunk(oc):
                d_ = pd[oc % 2]
                for fc in range(nf):
                    P.add("pe", (lambda fc: (lambda e: e.matmul(d_[:, 0:Tt], lhsT=D_[:, fc, oc * 128:(oc + 1) * 128], rhs=act[:, fc, 0:Tt], start=(fc == 0), stop=(fc == nf - 1))))(fc), [D_, act], [d_])
                if bi == 0:
                    P.add("act", lambda e: e.activation(out=acc[:, oc, 0:Tt], in_=d_[:, 0:Tt], func=AF.Copy), [d_], [acc])
                else:
                    P.add("dve", lambda e: e.tensor_tensor(out=acc[:, oc, 0:Tt], in0=d_[:, 0:Tt], in1=acc[:, oc, 0:Tt], op=ALU.add), [d_, acc], [acc])

            for oc in range(8):
                dchunk(oc)

        for bi in range(len(blocks)):
            block(bi)
            if bi == 0 and gi + 1 < len(groups):
                load_aux(gi + 1)
        for oc in range(8):
            P.add("dve", (lambda oc: (lambda e: e.scalar_tensor_tensor(out=acc[:, oc, 0:Tt], in0=acc[:, oc, 0:Tt], scalar=c.modv[:, l, 40 + oc, which:which + 1], in1=xt[:, oc, 0:Tt],
                                                                       op0=ALU.mult, op1=ALU.add)))(oc), [acc, xt, c.modv], [acc])
        P.dma("sp", XTv[:, :, t0:t0 + Tt], acc[:, :, 0:Tt], [acc], [P.R(("XT", b, t0))], "fxs")

    loadw(0)
    for gi, (b, t0, Tt) in enumerate(groups):
        group(gi, b, t0, Tt)


def stage_gdn_pre(m, l):
    P, c = m.P, m.c
    qin = [P.sb(f"gqin{i}", [128, 6, 514], F32) for i in range(2)]
    s = P.sb("gs", [128, 6, 512], F32)
    cacc = [P.sb(f"gcacc{i}", [128, 512], F32) for i in range(2)]
    sqq = P.sb("gsqq", [128, 512], BF16)
    rn = P.sb("grn", [128, 512], F32)
    kf = P.sb("gkf", [128, 2, 512], F32)
    qb = P.sb("gqb", [128, 2, 512], BF16)
    kb = P.sb("gkb", [128, 2, 512], BF16)
    tk = [P.sb(f"gtk{i}", [128, 512], F32) for i in range(2)]
    epsc = P.sb("gepsc", [128, 1], F32)
    P.add("pool", lambda e: e.memset(epsc[:, :], EPS), [], [epsc])
    ps = [P.ps(f"gps{i}", [128, 512]) for i in range(4)]
    tl = [(b, t0, Tt) for b in range(2) for (t0, Tt) in tiles_of_batch()]
    cnt = [0]

    def load(i):
        b, t0, Tt = tl[i]
        P.dma("sp", qin[i % 2][:, :, 0:Tt + 2], m.QKVp[b].rearrange("(c p) t -> p c t", p=128)[:, :, pc(t0) - 1:pc(t0) + Tt + 1], [P.R(("QKVp", b, t0))], [qin[i % 2]], ("gld", i % 2))

    def body(i, b, t0, Tt):
        x = qin[i % 2]

        def conv(j):
            a = cacc[j % 2]
            w = lambda k: c.vcol[:, l, R_CA + k * 6 + j:R_CA + k * 6 + j + 1]
            P.add("dve", lambda e: e.tensor_scalar(out=a[:, 0:Tt], in0=x[:, j, 0:Tt], scalar1=w(0), scalar2=None, op0=ALU.mult), [x, c.vcol], [a])
            P.add("dve", lambda e: e.scalar_tensor_tensor(out=a[:, 0:Tt], in0=x[:, j, 1:Tt + 1], scalar=w(1), in1=a[:, 0:Tt], op0=ALU.mult, op1=ALU.add), [x, a, c.vcol], [a])
            P.add("dve", lambda e: e.scalar_tensor_tensor(out=a[:, 0:Tt], in0=x[:, j, 2:Tt + 2], scalar=w(2), in1=a[:, 0:Tt], op0=ALU.mult, op1=ALU.add), [x, a, c.vcol], [a])
            P.add("act", lambda e: e.activation(out=s[:, j, 0:Tt], in_=a[:, 0:Tt], func=AF.Silu), [a], [s])

        for j in range(6):
            conv(j)

        def l2n(j):
            pss = ps[j % 2]
            P.add("act", lambda e: e.activation(out=sqq[:, 0:Tt], in_=s[:, j, 0:Tt], func=AF.Square), [s], [sqq])
            P.add("pe", lambda e: e.matmul(pss[:, 0:Tt], lhsT=c.bdiag[:, :], rhs=sqq[:, 0:Tt], start=True, stop=True), [sqq, c.bdiag], [pss])
            P.add("act", lambda e: e.activation(out=rn[:, 0:Tt], in_=pss[:, 0:Tt], func=AF.Sqrt, scale=1.0, bias=epsc[:, 0:1]), [pss, epsc], [rn])
            P.add("dve", lambda e: e.reciprocal(out=rn[:, 0:Tt], in_=rn[:, 0:Tt]), [rn], [rn])
            if j < 2:
                P.add("dve", lambda e: e.scalar_tensor_tensor(out=qb[:, j, 0:Tt], in0=s[:, j, 0:Tt], scalar=0.125, in1=rn[:, 0:Tt], op0=ALU.mult, op1=ALU.mult), [s, rn], [qb])
            else:
                P.add("dve", lambda e: e.tensor_tensor(out=kf[:, j - 2, 0:Tt], in0=s[:, j, 0:Tt], in1=rn[:, 0:Tt], op=ALU.mult), [s, rn], [kf])
                P.add("pool", lambda e: e.tensor_copy(out=kb[:, j - 2, 0:Tt], in_=kf[:, j - 2, 0:Tt]), [kf], [kb])

        for j in range(4):
            l2n(j)
        P.dma("sp", m.QG[b].rearrange("(c p) t -> p c t", p=128)[:, :, t0:t0 + Tt], qb[:, :, 0:Tt], [qb], [P.R(("QG", b, t0))], "gqb")
        P.dma("sp", m.KG[b].rearrange("(c p) t -> p c t", p=128)[:, :, t0:t0 + Tt], kb[:, :, 0:Tt], [kb], [P.R(("KG", b, t0))], "gkb")

        def tok(n):
            pt = ps[2 + n % 2]
            o = tk[cnt[0] % 2]
            cnt[0] += 1
            for j in range(2):
                P.add("pe", (lambda j: (lambda e: e.transpose(out=pt[:, j * 128:(j + 1) * 128], in_=kf[:, j, n * 128:(n + 1) * 128], identity=c.ident[:, :])))(j), [kf, c.ident], [pt])
                P.add("pe", (lambda j: (lambda e: e.transpose(out=pt[:, 256 + j * 128:256 + (j + 1) * 128], in_=s[:, 4 + j, n * 128:(n + 1) * 128], identity=c.ident[:, :])))(j), [s, c.ident], [pt])
            P.add("act", lambda e: e.activation(out=o[:, :], in_=pt[:, :], func=AF.Copy), [pt], [o])
            tt = t0 + n * 128
            P.dma("sp", m.KGt[b, tt:tt + 128, :], o[:, 0:256], [o], [P.R(("KGt", b, tt))], ("gtk", cnt[0] % 2))
            P.dma("sp", m.VGt[b, tt:tt + 128, :], o[:, 256:512], [o], [P.R(("VGt", b, tt))], ("gtk", cnt[0] % 2))

        for n in range(Tt // 128):
            tok(n)

    load(0)
    for i, (b, t0, Tt) in enumerate(tl):
        if i + 1 < len(tl):
            load(i + 1)
        body(i, b, t0, Tt)


def bc3(ap, n, w):
    return ap.unsqueeze(2).to_broadcast([ap.shape[0], n, w])


def bcm(ap, n):
    return ap.unsqueeze(1).to_broadcast([ap.shape[0], n, ap.shape[1]])


def stage_gdn_scan(m, l):
    P, c = m.P, m.c
    DCONST = {0: ((c.Ls, c.Ui, c.Ui, c.Us, c.Ui), list(range(0, T // CHK))),
              1: ((c.Us, c.Li, c.Li, c.Ls, c.Li), [1, 0] + list(range(T // CHK - 1, 1, -1)))}
    H = 4
    psr = [P.ps(f"spsr{i}", [128, 512]) for i in range(7)]
    ptr_sh = P.ps("sptr_sh", [128, 1024], BF16)
    pctr = [0]
    th = []
    gm4 = P.sb("gm4", [128, 7, H * CHK], BF16)
    for k_ in range(7):
        P.add("pool", (lambda k_: (lambda e: e.tensor_copy(out=gm4[:, k_, :].rearrange("p (h c) -> p h c", h=H), in_=bcm(c.gmask[:, k_, :], H))))(k_), [c.gmask], [gm4])
    P.add("pool", lambda e: e.tensor_scalar(out=gm4[:, 1:7, :], in0=gm4[:, 1:7, :], scalar1=-1.0, scalar2=None, op0=ALU.mult), [gm4], [gm4])
    for (b, d_) in ((0, 0), (1, 0), (0, 1), (1, 1)):
        t = M()
        t.b = b
        t.d = d_
        sb = lambda n, sh, dt, b=b, d_=d_: P.sb(f"s{b}{d_}{n}", sh, dt)
        t.kT = [sb(f"kT{i}", [64, H, CHK], BF16) for i in range(2)]
        t.qT = [sb(f"qT{i}", [64, H, CHK], BF16) for i in range(2)]
        t.kt = [sb(f"kt{i}", [128, H, 64], F32) for i in range(2)]
        t.vt = [sb(f"vt{i}", [128, H, 64], F32) for i in range(2)]
        t.gT = [sb(f"gT{i}", [16, CHK], F32) for i in range(2)]
        t.g = sb("g", [128, 16], F32)
        t.gc8 = sb("gc8", [128, 8], F32)
        t.ge = sb("ge", [128, 12], F32)
        t.E12 = sb("E12", [128, 12], F32)
        t.rarg = sb("rarg", [128, H * CHK], F32)
        t.Ee = sb("Ee", [128, H, CHK], F32)
        t.Di = sb("Di", [128, H, CHK], F32)
        t.Ds = sb("Ds", [128, H, CHK], F32)
        t.tmp = sb("tmp", [128, H, CHK], F32)
        t.X = [sb(f"X{i}", [128, H, CHK], BF16) for i in range(2)]
        t.XT = [sb(f"XT{i}", [128, H, CHK], BF16) for i in range(2)]
        t.PT = [sb(f"PT{i}", [128, H, CHK], BF16) for i in range(2)]
        t.IX = sb("IX", [128, H, CHK], BF16)
        t.U = sb("U", [128, H, CHK], BF16)
        t.UT = sb("UT", [128, H, CHK], BF16)
        t.QK = sb("QK", [128, H, CHK], BF16)
        t.t2 = sb("t2", [128, H, 64], F32)
        t.rb = sb("rb", [128, H, 64], BF16)
        t.vn = sb("vn", [128, H, 64], BF16)
        t.kd = sb("kd", [128, H, 64], BF16)
        t.o = [sb(f"o{i}", [128, H, 64], F32) for i in range(2)]
        t.S = sb("S", [64, H, 64], F32)
        t.Sb = sb("Sb", [64, H, 64], BF16)
        t.t4 = sb("t4", [64, H, 64], F32)
        t.ptr = ptr_sh
        P.add("pool", (lambda t: (lambda e: e.memset(t.S[:, :, :], 0.0)))(t), [], [t.S])
        P.add("pool", (lambda t: (lambda e: e.memset(t.Sb[:, :, :], 0.0)))(t), [], [t.Sb])
        th.append(t)

    def bank(t):
        p = psr[pctr[0] % 7]
        pctr[0] += 1
        return p

    def load(t, si):
        cc = DCONST[t.d][1][si]
        t0 = cc * CHK
        b, k = t.b, si % 2
        tl0 = 0 if t0 < CT else CT + ((t0 - CT) // 512) * 512
        P.dma("sp", t.kT[k][:, :, :], m.KG[b].rearrange("(h d) t -> d h t", d=64)[:, :, t0:t0 + CHK], [P.R(("KG", b, tl0))], [t.kT[k]], ("skT", b, t.d, k))
        P.dma("sp", t.qT[k][:, :, :], m.QG[b].rearrange("(h d) t -> d h t", d=64)[:, :, t0:t0 + CHK], [P.R(("QG", b, tl0))], [t.qT[k]], ("sqT", b, t.d, k))
        P.dma("sp", t.kt[k][:, :, :], m.KGt[b, t0:t0 + CHK, :].rearrange("p (h d) -> p h d", d=64), [P.R(("KGt", b, t0))], [t.kt[k]], ("skt", b, t.d, k))
        P.dma("sp", t.vt[k][:, :, :], m.VGt[b, t0:t0 + CHK, :].rearrange("p (h d) -> p h d", d=64), [P.R(("VGt", b, t0))], [t.vt[k]], ("svt", b, t.d, k))
        P.dma("sp", t.gT[k][:, :], m.G[b][:, t0:t0 + CHK], [P.R(("G", b, tl0))], [t.gT[k]], ("sgT", b, t.d, k))

    def step(t, si):
        d = t.d
        (LA, M1, INC, STR, LGC), order = DCONST[d]
        cc = order[si]
        t0 = cc * CHK
        k = si % 2
        kT, qT, kt, vt, gT = t.kT[k], t.qT[k], t.kt[k], t.vt[k], t.gT[k]
        la = t.g[:, d * 4:d * 4 + 4]
        be = t.g[:, 8 + d * 4:8 + d * 4 + 4]
        fs = []

        def f_gates():
            p = bank(t)
            P.add("pe", lambda e: e.transpose(out=p[:, 0:16], in_=gT[:, :], identity=c.ident[0:16, 0:16]), [gT, c.ident], [p])
            P.add("dve", lambda e: e.tensor_copy(out=t.g[:, :], in_=p[:, 0:16]), [p], [t.g])
            p2 = bank(t)
            P.add("pe", lambda e: e.matmul(p2[:, 0:4], lhsT=LGC[:, :], rhs=la, start=True, stop=True), [t.g, LGC], [p2])
            P.add("pe", lambda e: e.matmul(p2[:, 4:8], lhsT=c.onesf[:, :], rhs=la, start=True, stop=True), [t.g, c.onesf], [p2])
            P.add("dve", lambda e: e.tensor_copy(out=t.gc8[:, :], in_=p2[:, 0:8]), [p2], [t.gc8])
            P.add("pool", lambda e: e.tensor_copy(out=t.ge[:, 0:4], in_=t.gc8[:, 0:4]), [t.gc8], [t.ge])
            P.add("pool", lambda e: e.tensor_tensor(out=t.ge[:, 4:8], in0=t.gc8[:, 4:8], in1=t.gc8[:, 0:4], op=ALU.subtract), [t.gc8], [t.ge])
            P.add("pool", lambda e: e.tensor_copy(out=t.ge[:, 8:12], in_=t.gc8[:, 4:8]), [t.gc8], [t.ge])
            P.add("act", lambda e: e.activation(out=t.E12[:, :], in_=t.ge[:, :], func=AF.Exp), [t.ge], [t.E12])
            P.add("dve", lambda e: e.tensor_tensor(out=t.rarg[:, :].rearrange("p (h c) -> p h c", h=H), in0=bcm(M1[:, :], H), in1=bc3(la, H, CHK), op=ALU.mult), [M1, t.g], [t.rarg])
        fs.append(f_gates)

        def f_arg():
            p = bank(t)
            P.add("pe", lambda e: e.matmul(p[:, :], lhsT=LA[:, :], rhs=t.rarg[:, :], start=True, stop=True), [t.rarg, LA], [p])
            P.add("act", lambda e: e.activation(out=t.Ee[:, :, :], in_=p[:, :], func=AF.Exp), [p], [t.Ee])
            P.add("pool", lambda e: e.tensor_tensor(out=t.Di[:, :, :], in0=t.Ee[:, :, :], in1=bcm(INC[:, :], H), op=ALU.mult), [t.Ee, INC], [t.Di])
            P.add("pool", lambda e: e.tensor_tensor(out=t.Ds[:, :, :], in0=t.Ee[:, :, :], in1=bcm(STR[:, :], H), op=ALU.mult), [t.Ee, STR], [t.Ds])
        fs.append(f_arg)

        def f_kk():
            pk = bank(t)
            for h in range(H):
                P.add("pe", (lambda h: (lambda e: e.matmul(pk[:, h * CHK:(h + 1) * CHK], lhsT=kT[:, h, :], rhs=kT[:, h, :], start=True, stop=True)))(h), [kT], [pk])
            P.add("dve", lambda e: e.tensor_tensor(out=t.tmp[:, :, :], in0=pk[:, :], in1=t.Ds[:, :, :], op=ALU.mult), [pk, t.Ds], [t.tmp])
            P.add("pool", lambda e: e.tensor_tensor(out=t.XT[0][:, :, :], in0=t.tmp[:, :, :], in1=bc3(be, H, CHK), op=ALU.mult), [t.tmp, t.g], [t.XT[0]])
            pq = bank(t)
            for h in range(H):
                P.add("pe", (lambda h: (lambda e: e.matmul(pq[:, h * CHK:(h + 1) * CHK], lhsT=kT[:, h, :], rhs=qT[:, h, :], start=True, stop=True)))(h), [kT, qT], [pq])
            P.add("dve", lambda e: e.tensor_tensor(out=t.QK[:, :, :], in0=pq[:, :], in1=t.Di[:, :, :], op=ALU.mult), [pq, t.Di], [t.QK])
            for h in range(H):
                P.add("pe", (lambda h: (lambda e: e.transpose(out=t.ptr[:, h * CHK:(h + 1) * CHK], in_=t.XT[0][:, h, :], identity=c.identb[:, :])))(h), [t.XT[0], c.identb], [t.ptr])
            P.add("act", lambda e: e.activation(out=t.X[0][:, :, :], in_=t.ptr[:, 0:H * CHK], func=AF.Copy), [t.ptr], [t.X[0]])
        fs.append(f_kk)

        A_, AT_ = t.X[0], t.XT[0]
        Td, TdT = t.PT[0], t.PT[1]

        def f_base():
            P.add("pool", lambda e: e.tensor_tensor(out=t.IX[:, :, :], in0=A_[:, :, :], in1=bcm(c.gmask[:, 0, :], H), op=ALU.mult), [A_, c.gmask], [t.IX])
            P.add("pool", lambda e: e.tensor_tensor(out=Td[:, :, :], in0=bcm(c.identb[:, :], H), in1=t.IX[:, :, :], op=ALU.subtract), [t.IX, c.identb], [Td])
            P.add("pool", lambda e: e.tensor_tensor(out=t.IX[:, :, :], in0=AT_[:, :, :], in1=bcm(c.gmask[:, 0, :], H), op=ALU.mult), [AT_, c.gmask], [t.IX])
            P.add("pool", lambda e: e.tensor_tensor(out=TdT[:, :, :], in0=bcm(c.identb[:, :], H), in1=t.IX[:, :, :], op=ALU.subtract), [t.IX, c.identb], [TdT])
        fs.append(f_base)
        NMRG = 6

        def mk_merge(lv):
            def f():
                lastlv = (lv == NMRG)
                U, UT = t.U, t.UT
                if not lastlv:
                    pu = bank(t)
                    for h in range(H):
                        P.add("pe", (lambda h: (lambda e: e.matmul(pu[:, h * CHK:(h + 1) * CHK], lhsT=AT_[:, h, :], rhs=Td[:, h, :], start=True, stop=True)))(h), [AT_, Td], [pu])
                    P.add("dve", lambda e: e.tensor_tensor(out=U[:, :, :], in0=pu[:, :], in1=gm4[:, lv, :], op=ALU.mult), [pu, gm4], [U])
                put = bank(t)
                for h in range(H):
                    P.add("pe", (lambda h: (lambda e: e.matmul(put[:, h * CHK:(h + 1) * CHK], lhsT=A_[:, h, :], rhs=TdT[:, h, :], start=True, stop=True)))(h), [A_, TdT], [put])
                P.add("dve", lambda e: e.tensor_tensor(out=UT[:, :, :], in0=put[:, :], in1=gm4[:, lv, :], op=ALU.mult), [put, gm4], [UT])
                if not lastlv:
                    pw = bank(t)
                    for h in range(H):
                        P.add("pe", (lambda h: (lambda e: e.matmul(pw[:, h * CHK:(h + 1) * CHK], lhsT=TdT[:, h, :], rhs=c.identb[:, :], start=True, stop=False)))(h), [TdT, c.identb], [pw])
                        P.add("pe", (lambda h: (lambda e: e.matmul(pw[:, h * CHK:(h + 1) * CHK], lhsT=TdT[:, h, :], rhs=U[:, h, :], start=False, stop=True)))(h), [TdT, U], [pw])
                pwt = bank(t)
                for h in range(H):
                    P.add("pe", (lambda h: (lambda e: e.matmul(pwt[:, h * CHK:(h + 1) * CHK], lhsT=Td[:, h, :], rhs=c.identb[:, :], start=True, stop=False)))(h), [Td, c.identb], [pwt])
                    P.add("pe", (lambda h: (lambda e: e.matmul(pwt[:, h * CHK:(h + 1) * CHK], lhsT=Td[:, h, :], rhs=UT[:, h, :], start=False, stop=True)))(h), [Td, UT], [pwt])
                if not lastlv:
                    P.add("act", lambda e: e.activation(out=Td[:, :, :], in_=pw[:, :], func=AF.Copy), [pw], [Td])
                P.add("act", lambda e: e.activation(out=TdT[:, :, :], in_=pwt[:, :], func=AF.Copy), [pwt], [TdT])
            return f
        for lv in range(1, NMRG + 1):
            fs.append(mk_merge(lv))
        PTf = TdT
        egc = t.E12[:, 0:4]

        def f_scan1():
            p = bank(t)
            for h in range(H):
                P.add("pe", (lambda h: (lambda e: e.matmul(p[:, h * 64:(h + 1) * 64], lhsT=kT[:, h, :], rhs=t.Sb[:, h, :], start=True, stop=True)))(h), [kT, t.Sb], [p])
            P.add("dve", lambda e: e.tensor_tensor(out=t.t2[:, :, :], in0=p[:, 0:H * 64], in1=bc3(egc, H, 64), op=ALU.mult), [p, t.E12], [t.t2])
            P.add("pool", lambda e: e.tensor_tensor(out=t.rb[:, :, :], in0=vt[:, :, :], in1=t.t2[:, :, :], op=ALU.subtract), [vt, t.t2], [t.rb])
            P.add("pool", lambda e: e.tensor_tensor(out=t.kd[:, :, :], in0=kt[:, :, :], in1=bc3(t.E12[:, 4:8], H, 64), op=ALU.mult), [kt, t.E12], [t.kd])
        fs.append(f_scan1)

        def f_scan2():
            p = bank(t)
            for h in range(H):
                P.add("pe", (lambda h: (lambda e: e.matmul(p[:, h * 64:(h + 1) * 64], lhsT=PTf[:, h, :], rhs=t.rb[:, h, :], start=True, stop=True)))(h), [PTf, t.rb], [p])
            P.add("dve", lambda e: e.tensor_tensor(out=t.vn[:, :, :], in0=p[:, 0:H * 64], in1=bc3(be, H, 64), op=ALU.mult), [p, t.g], [t.vn])
        fs.append(f_scan2)

        def f_scan3():
            p1 = bank(t)
            for h in range(H):
                P.add("pe", (lambda h: (lambda e: e.matmul(p1[:, h * 64:(h + 1) * 64], lhsT=qT[:, h, :], rhs=t.Sb[:, h, :], start=True, stop=True)))(h), [qT, t.Sb], [p1])
            o = t.o[si % 2]
            P.add("dve", lambda e: e.tensor_tensor(out=t.t2[:, :, :], in0=p1[:, 0:H * 64], in1=bc3(egc, H, 64), op=ALU.mult), [p1, t.E12], [t.t2])
            p2 = bank(t)
            for h in range(H):
                P.add("pe", (lambda h: (lambda e: e.matmul(p2[:, h * 64:(h + 1) * 64], lhsT=t.QK[:, h, :], rhs=t.vn[:, h, :], start=True, stop=True)))(h), [t.QK, t.vn], [p2])
            P.add("dve", lambda e: e.tensor_tensor(out=o[:, :, :], in0=p2[:, 0:H * 64], in1=t.t2[:, :, :], op=ALU.add), [p2, t.t2], [o])
            P.dma("sp", m.OG[d, t.b, t0:t0 + CHK, :].rearrange("p (h d) -> p h d", d=64), o[:, :, :], [o], [P.R(("OG", d, t.b, t0))], ("so", t.b, t.d, si % 2))
            p3 = bank(t)
            for h in range(H):
                P.add("pe", (lambda h: (lambda e: e.matmul(p3[0:64, h * 64:(h + 1) * 64], lhsT=t.kd[:, h, :], rhs=t.vn[:, h, :], start=True, stop=True)))(h), [t.kd, t.vn], [p3])
            P.add("pool", lambda e: e.tensor_tensor(out=t.t4[:, :, :], in0=t.S[:, :, :], in1=bc3(t.E12[0:64, 8:12], H, 64), op=ALU.mult), [t.S, t.E12], [t.t4])
            P.add("dve", lambda e: e.tensor_tensor(out=t.S[:, :, :], in0=p3[0:64, 0:H * 64], in1=t.t4[:, :, :], op=ALU.add), [p3, t.t4], [t.S])
            P.add("act", lambda e: e.activation(out=t.Sb[:, :, :], in_=t.S[:, :, :], func=AF.Copy), [t.S], [t.Sb])
        fs.append(f_scan3)
        return fs

    nst = (T // CHK) if GDN_STEPS is None else GDN_STEPS
    for t in th:
        load(t, 0)
    for si in range(nst):
        for t in th:
            if si + 1 < nst:
                load(t, si + 1)
        fss = [step(t, si)[:GSUB] for t in th]
        for grp in zip(*fss):
            for f_ in grp:
                f_()


def stage_gdn_fin(m, l, last):
    P, c = m.P, m.c
    tl = [(b, t0, Tt) for b in range(2) for (t0, Tt) in tiles_of_batch() if not (last and t0 < CT)]
    of = [P.sb(f"nof{i}", [128, 4, 256], F32) for i in range(2)]
    ob = [P.sb(f"nob{i}", [128, 4, 256], F32) for i in range(2)]
    zz = [P.sb(f"nzz{i}", [128, 4, 256], F32) for i in range(2)]
    sq = P.sb("nsq", [128, 4, 256], F32)
    ss = P.sb("nss", [128, 16], F32)
    ya = P.sb("nya", [128, 4, 256], F32)
    st = P.sb("nst", [128, 2, 512], BF16)
    epsc = P.sb("nepsc", [128, 1], F32)
    P.add("pool", lambda e: e.memset(epsc[:, :], EPS), [], [epsc])
    ps = [P.ps(f"nps{i}", [128, 512]) for i in range(2)]

    def load(i):
        b, t0, Tt = tl[i]
        ns = Tt // 128
        k = i % 2
        P.dma("sp", of[k][:, 0:ns, :], m.OG[0, b, t0:t0 + Tt, :].rearrange("(n p) f -> p n f", p=128), [P.R(("OG", 0, b, t0 + n * 128)) for n in range(ns)], [of[k]], ("nof", k))
        P.dma("sp", ob[k][:, 0:ns, :], m.OG[1, b, t0:t0 + Tt, :].rearrange("(n p) f -> p n f", p=128), [P.R(("OG", 1, b, t0 + n * 128)) for n in range(ns)], [ob[k]], ("nob", k))
        P.dma("sp", zz[k][:, 0:ns, :], m.Zs[b, t0:t0 + Tt, :].rearrange("(n p) f -> p n f", p=128), [P.R(("Zs", b, t0))], [zz[k]], ("nzz", k))

    def body(i, b, t0, Tt):
        ns = Tt // 128
        k = i % 2
        o_, b_, z_ = of[k], ob[k], zz[k]
        P.add("dve", lambda e: e.tensor_tensor(out=o_[:, 0:ns, :], in0=o_[:, 0:ns, :], in1=b_[:, 0:ns, :], op=ALU.add), [o_, b_], [o_])
        P.add("pool", lambda e: e.tensor_tensor(out=sq[:, 0:ns, :], in0=o_[:, 0:ns, :], in1=o_[:, 0:ns, :], op=ALU.mult), [o_], [sq])
        P.add("dve", lambda e: e.tensor_reduce(out=ss[:, 0:ns * 4], in_=sq[:, 0:ns, :].rearrange("p n (h d) -> p (n h) d", d=64), axis=AX.X, op=ALU.add), [sq], [ss])
        P.add("act", lambda e: e.activation(out=ss[:, 0:ns * 4], in_=ss[:, 0:ns * 4], func=AF.Sqrt, scale=1.0 / 64, bias=epsc[:, 0:1]), [ss, epsc], [ss])
        P.add("dve", lambda e: e.reciprocal(out=ss[:, 0:ns * 4], in_=ss[:, 0:ns * 4]), [ss], [ss])
        ov = o_[:, 0:ns, :].rearrange("p n (h d) -> p (n h) d", d=64)
        yv = ya[:, 0:ns, :].rearrange("p n (h d) -> p (n h) d", d=64)
        P.add("dve", lambda e: e.tensor_tensor(out=yv, in0=ov, in1=bc3(ss[:, 0:ns * 4], ns * 4, 64), op=ALU.mult), [o_, ss], [ya])
        P.add("pool", lambda e: e.tensor_tensor(out=yv, in0=yv, in1=bcm(c.gdng[:, l, :], ns * 4), op=ALU.mult), [ya, c.gdng], [ya])
        P.add("dve", lambda e: e.tensor_tensor(out=ya[:, 0:ns, :], in0=ya[:, 0:ns, :], in1=z_[:, 0:ns, :], op=ALU.mult), [ya, z_], [ya])
        for j in range(2):
            for n in range(ns):
                P.add("pe", (lambda j, n: (lambda e: e.transpose(out=ps[j][:, n * 128:(n + 1) * 128], in_=ya[:, n, j * 128:(j + 1) * 128], identity=c.ident[:, :])))(j, n), [ya, c.ident], [ps[j]])
            if j == 0:
                P.add("act", lambda e: e.activation(out=st[:, 0, 0:Tt], in_=ps[0][:, 0:Tt], func=AF.Copy), [ps[0]], [st])
            else:
                P.add("dve", lambda e: e.tensor_copy(out=st[:, 1, 0:Tt], in_=ps[1][:, 0:Tt]), [ps[1]], [st])
        P.dma("sp", m.YT[b][0:256, :].rearrange("(j p) t -> p j t", p=128)[:, :, t0:t0 + Tt], st[:, :, 0:Tt], [st], [P.R(("YT", b, t0))], "nst")

    load(0)
    for i, (b, t0, Tt) in enumerate(tl):
        if i + 1 < len(tl):
            load(i + 1)
        body(i, b, t0, Tt)
```
